# Optimizing a Trainium2 kernel written in Bass

```python
import math
import jax, jax.numpy as jnp
from jax import lax
import numpy as np

D_MODEL = 1024
BATCH = 4
SEQ = 4096
DEPTH = 2

N_A_LAYERS = DEPTH // 2
N_B_LAYERS = DEPTH - N_A_LAYERS
N_DENSE = (DEPTH + 1) // 2
N_MOE = DEPTH // 2

CHUNK = 128
GMLP_FFN = 6 * D_MODEL
GMLP_HALF = GMLP_FFN // 2
GMLP_GROUPS = 8
GMLP_GROUP_DIM = GMLP_HALF // GMLP_GROUPS

N_HEADS = 16
N_KV_HEADS = 4
HEAD_DIM = 64
KV_REP = N_HEADS // N_KV_HEADS
WINDOW = 128
ROPE_THETA = 10000.0

D_FF_DENSE = 2816
N_EXPERTS = 8
TOP_K = 2
D_FF_EXPERT = 3584

PLE_DIM = 256

EPS = 1e-6
MASK_VALUE = -1e30

kernel_name = "yoco_gmlp_swa_sink_moe_trunk"


def rms_norm(x, g):
    xf = x.astype(jnp.float32)
    y = xf * lax.rsqrt(jnp.mean(xf * xf, axis=-1, keepdims=True) + EPS)
    return (y * g.astype(jnp.float32)).astype(x.dtype)


def layer_norm(x, g, b):
    xf = x.astype(jnp.float32)
    mu = jnp.mean(xf, axis=-1, keepdims=True)
    xc = xf - mu
    var = jnp.mean(xc * xc, axis=-1, keepdims=True)
    y = xc * lax.rsqrt(var + EPS)
    return (y * g.astype(jnp.float32) + b.astype(jnp.float32)).astype(x.dtype)


def rope(t):
    S, Dh = t.shape[1], t.shape[-1]
    freqs = ROPE_THETA ** (-jnp.arange(0, Dh, 2, dtype=jnp.float32) / Dh)
    ang = jnp.arange(S, dtype=jnp.float32)[:, None] * freqs[None, :]
    cos = jnp.cos(ang)[None, :, None, :]
    sin = jnp.sin(ang)[None, :, None, :]
    tf = t.astype(jnp.float32)
    t1, t2 = tf[..., : Dh // 2], tf[..., Dh // 2:]
    out = jnp.concatenate([t1 * cos - t2 * sin, t2 * cos + t1 * sin], axis=-1)
    return out.astype(t.dtype)


def gmlp_mixer(h, w_in, b_in, ln_g, ln_b, w_s, b_s, w_out, b_out):
    B, S, _ = h.shape
    n_chunks = S // CHUNK
    uv = jax.nn.gelu(h @ w_in + b_in)
    u, v = uv[..., :GMLP_HALF], uv[..., GMLP_HALF:]
    v = layer_norm(v, ln_g, ln_b)
    v = v.reshape(B, n_chunks, CHUNK, GMLP_GROUPS, GMLP_GROUP_DIM)
    causal = jnp.tril(jnp.ones((CHUNK, CHUNK), dtype=bool))
    ws = jnp.where(causal[None], w_s, jnp.zeros((), w_s.dtype))
    mixed = jnp.einsum('gts,bcsgd->bctgd', ws, v)
    mixed = mixed + jnp.transpose(b_s)[None, None, :, :, None]
    gated = u * mixed.reshape(B, S, GMLP_HALF)
    return gated @ w_out + b_out


def shared_kv(h, kv_norm_g, w_kv, b_kv):
    B, S, _ = h.shape
    n_blocks = S // WINDOW
    kv = rms_norm(h, kv_norm_g) @ w_kv + b_kv
    k = kv[..., : N_KV_HEADS * HEAD_DIM].reshape(B, S, N_KV_HEADS, HEAD_DIM)
    v = kv[..., N_KV_HEADS * HEAD_DIM:].reshape(B, S, N_KV_HEADS, HEAD_DIM)
    k = rope(k)

    def band(t):
        tp = jnp.pad(t, ((0, 0), (WINDOW, 0), (0, 0), (0, 0)))
        tp = tp.reshape(B, n_blocks + 1, WINDOW, N_KV_HEADS, HEAD_DIM)
        return jnp.concatenate([tp[:, :-1], tp[:, 1:]], axis=2)

    return band(k), band(v)


def swa_sink_attention(h, w_q, b_q, sinks, w_o, b_o, k_band, v_band):
    B, S, _ = h.shape
    n_blocks = S // WINDOW
    q = (h @ w_q + b_q).reshape(B, S, N_HEADS, HEAD_DIM)
    q = rope(q).reshape(B, n_blocks, WINDOW, N_KV_HEADS, KV_REP, HEAD_DIM)
    scores = jnp.einsum('bcqkrd,bcjkd->bckrqj', q, k_band).astype(jnp.float32)
    scores = scores * (1.0 / math.sqrt(HEAD_DIM))
    qi = jnp.arange(WINDOW)[:, None]
    kj = jnp.arange(2 * WINDOW)[None, :]
    in_window = (kj > qi) & (kj <= qi + WINDOW)
    key_pos = jnp.arange(n_blocks)[:, None] * WINDOW + jnp.arange(2 * WINDOW)[None, :] - WINDOW
    mask = in_window[None] & (key_pos >= 0)[:, None, :]
    scores = jnp.where(mask[None, :, None, None], scores, MASK_VALUE)
    sink = sinks.astype(jnp.float32).reshape(N_KV_HEADS, KV_REP)[None, None, :, :, None, None]
    sink = jnp.broadcast_to(sink, scores.shape[:-1] + (1,))
    probs = jax.nn.softmax(jnp.concatenate([scores, sink], axis=-1), axis=-1)[..., :-1]
    out = jnp.einsum('bckrqj,bcjkd->bcqkrd', probs.astype(v_band.dtype), v_band)
    out = out.reshape(B, S, N_HEADS * HEAD_DIM)
    return out @ w_o + b_o


def swiglu(h, w_gate, w_up, w_down):
    return (jax.nn.silu(h @ w_gate) * (h @ w_up)) @ w_down


def moe_swiglu(h, w_router, w_gate, w_up, w_down):
    B, S, D = h.shape
    t = h.reshape(B * S, D)
    logits = (t @ w_router).astype(jnp.float32)
    top_vals, top_idx = lax.top_k(logits, TOP_K)
    top_w = jax.nn.softmax(top_vals, axis=-1)
    combine = jnp.sum(jax.nn.one_hot(top_idx, N_EXPERTS, dtype=jnp.float32) * top_w[..., None], axis=1)
    combine = combine.astype(t.dtype)
    y = jnp.zeros_like(t)
    for e in range(N_EXPERTS):
        y = y + combine[:, e:e + 1] * swiglu(t, w_gate[e], w_up[e], w_down[e])
    return y.reshape(B, S, D)


def per_layer_embedding(h, p_i, w_proj, norm_g, w_gate):
    gate = jax.nn.sigmoid(rms_norm(h, norm_g) @ w_gate)
    return (p_i @ w_proj) * gate


def setup_inputs(seed: int = 0) -> dict:
    key = jax.random.key(seed)
    ks = iter(jax.random.split(key, 40))
    f32 = jnp.float32

    def nrm(shape, scale):
        return jax.random.normal(next(ks), shape, f32) * scale

    def gain(shape):
        return 1.0 + nrm(shape, 0.05)

    D = D_MODEL
    QW = N_HEADS * HEAD_DIM
    KVW = 2 * N_KV_HEADS * HEAD_DIM
    return {
        "x": nrm((BATCH, SEQ, D), 1.0),
        "p": nrm((DEPTH, BATCH, SEQ, PLE_DIM), 1.0),
        "mix_norm_g": gain((DEPTH, D)),
        "ffn_norm_g": gain((DEPTH, D)),
        "gmlp_w_in": nrm((N_A_LAYERS, D, GMLP_FFN), D ** -0.5),
        "gmlp_b_in": nrm((N_A_LAYERS, GMLP_FFN), 0.02),
        "gmlp_ln_g": gain((N_A_LAYERS, GMLP_HALF)),
        "gmlp_ln_b": nrm((N_A_LAYERS, GMLP_HALF), 0.02),
        "gmlp_w_s": nrm((N_A_LAYERS, GMLP_GROUPS, CHUNK, CHUNK), 0.5 * CHUNK ** -0.5),
        "gmlp_b_s": gain((N_A_LAYERS, GMLP_GROUPS, CHUNK)),
        "gmlp_w_out": nrm((N_A_LAYERS, GMLP_HALF, D), GMLP_HALF ** -0.5),
        "gmlp_b_out": nrm((N_A_LAYERS, D), 0.02),
        "kv_norm_g": gain((D,)),
        "w_kv": nrm((D, KVW), D ** -0.5),
        "b_kv": nrm((KVW,), 0.02),
        "attn_w_q": nrm((N_B_LAYERS, D, QW), D ** -0.5),
        "attn_b_q": nrm((N_B_LAYERS, QW), 0.02),
        "attn_sinks": nrm((N_B_LAYERS, N_HEADS), 0.5),
        "attn_w_o": nrm((N_B_LAYERS, QW, D), QW ** -0.5),
        "attn_b_o": nrm((N_B_LAYERS, D), 0.02),
        "ffn_w_gate": nrm((N_DENSE, D, D_FF_DENSE), D ** -0.5),
        "ffn_w_up": nrm((N_DENSE, D, D_FF_DENSE), D ** -0.5),
        "ffn_w_down": nrm((N_DENSE, D_FF_DENSE, D), D_FF_DENSE ** -0.5),
        "moe_w_router": nrm((N_MOE, D, N_EXPERTS), D ** -0.5),
        "moe_w_gate": nrm((N_MOE, N_EXPERTS, D, D_FF_EXPERT), D ** -0.5),
        "moe_w_up": nrm((N_MOE, N_EXPERTS, D, D_FF_EXPERT), D ** -0.5),
        "moe_w_down": nrm((N_MOE, N_EXPERTS, D_FF_EXPERT, D), D_FF_EXPERT ** -0.5),
        "ple_w_proj": nrm((DEPTH, PLE_DIM, D), PLE_DIM ** -0.5),
        "ple_norm_g": gain((DEPTH, D)),
        "ple_w_gate": nrm((DEPTH, D, D), D ** -0.5),
        "final_norm_g": gain((D,)),
    }


def reference(x, p, mix_norm_g, ffn_norm_g,
              gmlp_w_in, gmlp_b_in, gmlp_ln_g, gmlp_ln_b, gmlp_w_s, gmlp_b_s, gmlp_w_out, gmlp_b_out,
              kv_norm_g, w_kv, b_kv,
              attn_w_q, attn_b_q, attn_sinks, attn_w_o, attn_b_o,
              ffn_w_gate, ffn_w_up, ffn_w_down,
              moe_w_router, moe_w_gate, moe_w_up, moe_w_down,
              ple_w_proj, ple_norm_g, ple_w_gate,
              final_norm_g):
    k_band, v_band = None, None
    for i in range(DEPTH):
        h = rms_norm(x, mix_norm_g[i])
        if i < N_A_LAYERS:
            a = i
            x = x + gmlp_mixer(h, gmlp_w_in[a], gmlp_b_in[a], gmlp_ln_g[a], gmlp_ln_b[a],
                               gmlp_w_s[a], gmlp_b_s[a], gmlp_w_out[a], gmlp_b_out[a])
        else:
            b = i - N_A_LAYERS
            if b == 0:
                k_band, v_band = shared_kv(x, kv_norm_g, w_kv, b_kv)
            x = x + swa_sink_attention(h, attn_w_q[b], attn_b_q[b], attn_sinks[b],
                                       attn_w_o[b], attn_b_o[b], k_band, v_band)
        hn = rms_norm(x, ffn_norm_g[i])
        if i % 2 == 0:
            j = i // 2
            x = x + swiglu(hn, ffn_w_gate[j], ffn_w_up[j], ffn_w_down[j])
        else:
            j = i // 2
            x = x + moe_swiglu(hn, moe_w_router[j], moe_w_gate[j], moe_w_up[j], moe_w_down[j])
        x = x + per_layer_embedding(x, p[i], ple_w_proj[i], ple_norm_g[i], ple_w_gate[i])
    return rms_norm(x, final_norm_g)
```

```python
import math
import os
from contextlib import ExitStack

import numpy as np
import concourse.bass as bass
import concourse.mybir as mybir
from concourse.bass_utils import run_bass_kernel_spmd

F32 = mybir.dt.float32
BF16 = mybir.dt.bfloat16
I32 = mybir.dt.int32
AF = mybir.ActivationFunctionType
ALU = mybir.AluOpType
AX = mybir.AxisListType

NCORES = 8
D = 1024
SEQ = 4096
OWN = 2048
HALO = 128
NF = OWN + HALO
GF = 6144
GH = 3072
DFF = 2816
NE = 8
DFE = 3584
PLE = 256
EPS = 1e-6
NEG = -1e30

ENGS = ("pe", "act", "dve", "pool", "sp")


class Op:
    __slots__ = ("eng", "emit", "deps", "sem", "seq", "signal", "is_dma")

    def __init__(self, eng, emit, is_dma):
        self.eng = eng
        self.emit = emit
        self.deps = []
        self.sem = None
        self.seq = 0
        self.signal = False
        self.is_dma = is_dma


class Prog:
    def __init__(self):
        self.ops = {e: [] for e in ENGS}
        self.last_write = {}
        self.readers = {}
        self.dma_tot = {}
        self.epoch_op = None
        self.shared = {"dc%d" % i for i in range(8)}

    def add(self, eng, emit, reads=(), writes=(), dma_sem=None):
        op = Op(eng, emit, dma_sem is not None)
        deps = {}
        for k in reads:
            w = self.last_write.get(k)
            if w is not None:
                deps[id(w)] = w
        for k in writes:
            w = self.last_write.get(k)
            if w is not None:
                deps[id(w)] = w
            for r in self.readers.get(k, ()):
                deps[id(r)] = r
        if self.epoch_op is not None:
            deps[id(self.epoch_op)] = self.epoch_op
        for k in reads:
            self.readers.setdefault(k, []).append(op)
        for k in writes:
            self.last_write[k] = op
            self.readers[k] = []
        for d in deps.values():
            if d is op:
                continue
            if (not d.is_dma) and d.eng == eng and eng in ("pe", "sp"):
                continue
            if d.is_dma and d.sem in self.shared:
                op.deps.append((d, self.dma_tot[d.sem]))
            else:
                op.deps.append((d, None))
            d.signal = True
        if dma_sem is not None:
            op.sem = dma_sem
            self.dma_tot[dma_sem] = self.dma_tot.get(dma_sem, 0) + 16
            op.seq = self.dma_tot[dma_sem]
        self.ops[eng].append(op)
        return op

    def barrier(self, emit):
        op = Op("dve", emit, False)
        seen = {}
        for e in ENGS:
            lst = self.ops[e]
            last_nd = None
            for o in lst:
                if o.is_dma:
                    seen[id(o)] = o
                else:
                    last_nd = o
            if last_nd is not None:
                seen[id(last_nd)] = last_nd
        newest = {}
        for o in seen.values():
            if o.is_dma:
                if o.sem not in newest or newest[o.sem].seq < o.seq:
                    newest[o.sem] = o
        for o in seen.values():
            if o.is_dma and newest[o.sem] is not o:
                continue
            op.deps.append((o, None))
            o.signal = True
        self.ops["dve"].append(op)
        self.epoch_op = op
        self.last_write = {}
        self.readers = {}
        return op

    def emit_all(self, nc, sems, final_waits=()):
        for e in ENGS:
            c = 0
            for op in self.ops[e]:
                if op.is_dma:
                    continue
                if op.signal:
                    c += 1
                    op.seq = c
                    op.sem = e

        def run(e, h):
            waited = {}
            for op in self.ops[e]:
                for d, ov in op.deps:
                    v = d.seq if ov is None else ov
                    if waited.get(d.sem, 0) < v:
                        h.wait_ge(sems[d.sem], v)
                        waited[d.sem] = v
                inst = op.emit(h)
                if op.is_dma:
                    inst.then_inc(sems[op.sem], 16)
                elif op.signal:
                    inst.then_inc(sems[op.sem], 1)
            if e == "sp":
                for d in final_waits:
                    if waited.get(d.sem, 0) < d.seq:
                        h.wait_ge(sems[d.sem], d.seq)
                        waited[d.sem] = d.seq

        with nc.Block() as block:
            @block.tensor
            def _(h):
                run("pe", h)

            @block.scalar
            def _(h):
                run("act", h)

            @block.vector
            def _(h):
                run("dve", h)

            @block.gpsimd
            def _(h):
                run("pool", h)

            @block.sync
            def _(h):
                run("sp", h)


WEIGHT_SPECS = [
    ("mix_norm_g", [2, D]), ("ffn_norm_g", [2, D]),
    ("gmlp_w_in", [1, D, GF]), ("gmlp_b_in", [1, GF]), ("gmlp_ln_g", [1, GH]), ("gmlp_ln_b", [1, GH]),
    ("gmlp_w_s", [1, 8, 128, 128]), ("gmlp_b_s", [1, 8, 128]), ("gmlp_w_out", [1, GH, D]),
    ("gmlp_b_out", [1, D]), ("kv_norm_g", [D]), ("w_kv", [D, 512]), ("b_kv", [512]),
    ("attn_w_q", [1, D, D]), ("attn_b_q", [1, D]), ("attn_sinks", [1, 16]), ("attn_w_o", [1, D, D]),
    ("attn_b_o", [1, D]), ("ffn_w_gate", [1, D, DFF]), ("ffn_w_up", [1, D, DFF]),
    ("ffn_w_down", [1, DFF, D]), ("moe_w_router", [1, D, NE]), ("moe_w_gate", [1, NE, D, DFE]),
    ("moe_w_up", [1, NE, D, DFE]), ("moe_w_down", [1, NE, DFE, D]), ("ple_w_proj", [2, PLE, D]),
    ("ple_norm_g", [2, D]), ("ple_w_gate", [2, D, D]), ("final_norm_g", [D]),
]


def build(stop_after=99, final_norm=True):
    nc = bass.Bass("TRN2", target_bir_lowering=False)
    W = {}
    for name, shp in WEIGHT_SPECS:
        W[name] = nc.dram_tensor(name, shp, F32, kind="ExternalInput").ap()
    x_in = nc.dram_tensor("xc", [NF, D], F32, kind="ExternalInput").ap()
    p0_in = nc.dram_tensor("p0c", [NF, PLE], F32, kind="ExternalInput").ap()
    p1_in = nc.dram_tensor("p1c", [OWN, PLE], F32, kind="ExternalInput").ap()
    pos_in = nc.dram_tensor("pos", [NF], F32, kind="ExternalInput").ap()
    hm_in = nc.dram_tensor("hm", [128, 128], F32, kind="ExternalInput").ap()
    y_out = nc.dram_tensor("y", [OWN, D], F32, kind="ExternalOutput").ap()

    P = Prog()
    es = ExitStack()
    ARENA_B = 206 * 1024
    arena = es.enter_context(nc.sbuf_tensor("arena", [128, ARENA_B // 2], BF16))
    PS = es.enter_context(nc.psum_tensor("ps", [128, 4096], F32))
    sem_names = list(ENGS) + ["dx", "dx2", "dp", "dc", "dout", "dout2"] + ["dc%d" % i for i in range(8)] + ["wa%d" % i for i in range(4)] + \
        ["wb%d" % i for i in range(4)] + ["wc%d" % i for i in range(4)] + ["wr0", "wr1", "wr2", "wr3", "xwa0", "xwa1", "xwa2", "xwc0", "xwc1", "hwa0", "hwa1", "hwa2", "hwc0", "hwc1", "xwb0", "xwb1", "xwb2", "hwb0", "hwb1", "hwb2", "xwc2", "hwc2"]
    sems = {n: es.enter_context(nc.semaphore(n)) for n in sem_names}

    state = {"off": 0, "mark": 0}

    def alloc(shape, dt, parts=128):
        nb = int(np.prod(shape[1:])) * (4 if dt in (F32, I32) else 2)
        nb = (nb + 63) // 64 * 64
        off = state["off"]
        assert off + nb <= ARENA_B, ("arena overflow", off, nb)
        state["off"] = off + nb
        v = arena[:, off // 2:(off + nb) // 2]
        if dt in (F32, I32):
            v = v.bitcast(dt)
        n = int(np.prod(shape[1:]))
        v = v[:, 0:n]
        if len(shape) == 3:
            v = v.rearrange("p (a b) -> p a b", a=shape[1])
        elif len(shape) == 4:
            v = v.rearrange("p (a b c) -> p a b c", a=shape[1], b=shape[2])
        if shape[0] < 128:
            v = v[0:shape[0]]
        return v

    def bank(i, n=1):
        return PS[:, 512 * i:512 * (i + n)]

    def bankb(i):
        return PS[:, 512 * i:512 * (i + 1)].bitcast(BF16)

    def BK(i, n=1):
        return ["B%d" % j for j in range(i, i + n)]

    def MM(out, lhsT, rhs, start, stop, r, w):
        P.add("pe", lambda h: h.matmul(out, lhsT, rhs, start=start, stop=stop), reads=r, writes=w)

    def TR(out, in_, ident, r, w):
        P.add("pe", lambda h: h.transpose(out=out, in_=in_, identity=ident), reads=r, writes=w)

    def ACT(out, in_, func, r, w, bias=None, scale=1.0, accum=None):
        kw = {}
        if bias is not None:
            kw["bias"] = bias
        if accum is not None:
            kw["accum_out"] = accum
        P.add("act", lambda h: h.activation(out=out, in_=in_, func=func, scale=scale, **kw), reads=r, writes=w)

    def TT(eng, out, in0, in1, op, r, w):
        P.add(eng, lambda h: h.tensor_tensor(out=out, in0=in0, in1=in1, op=op), reads=r, writes=w)

    def TS(eng, out, in0, s1, op0, r, w, s2=None, op1=None):
        if op1 is None:
            P.add(eng, lambda h: h.tensor_scalar(out=out, in0=in0, scalar1=s1, scalar2=None, op0=op0), reads=r, writes=w)
        else:
            P.add(eng, lambda h: h.tensor_scalar(out=out, in0=in0, scalar1=s1, scalar2=s2, op0=op0, op1=op1), reads=r, writes=w)

    def STT(eng, out, in0, scalar, in1, op0, op1, r, w):
        P.add(eng, lambda h: h.scalar_tensor_tensor(out=out, in0=in0, scalar=scalar, in1=in1, op0=op0, op1=op1),
              reads=r, writes=w)

    def CP(eng, out, in_, r, w):
        if eng == "act":
            P.add("act", lambda h: h.copy(out=out, in_=in_), reads=r, writes=w)
        else:
            P.add(eng, lambda h: h.tensor_copy(out=out, in_=in_), reads=r, writes=w)

    def MS(eng, ap, val, w):
        P.add(eng, lambda h: h.memset(ap, val), writes=w)

    dc_rot = [0]

    def DMA(eng, out, in_, sem, r, w, slow=False):
        if sem == "dc":
            sem = "dc%d" % (dc_rot[0] % 8)
            dc_rot[0] += 1
        if slow:
            return P.add(eng, lambda h: h.dma_start(out=out, in_=in_, allow_slow_non_contiguous=True),
                         reads=r, writes=w, dma_sem=sem)
        return P.add(eng, lambda h: h.dma_start(out=out, in_=in_), reads=r, writes=w, dma_sem=sem)

    def RECIP(out, in_, r, w):
        P.add("dve", lambda h: h.reciprocal(out=out, in_=in_), reads=r, writes=w)

    def epoch():
        dummy = alloc_dummy[0]
        P.barrier(lambda h: h.memset(dummy, 0.0))
        state["off"] = state["mark"]

    xT = alloc([128, 8, NF], F32)
    ident32 = alloc([128, 128], F32)
    ones32 = alloc([128, 128], F32)
    identb = alloc([128, 128], BF16)
    onesb = alloc([128, 128], BF16)
    epst = alloc([128, 1], F32)
    pit = alloc([128, 1], F32)
    dummy_t = alloc([128, 16], F32)
    alloc_dummy = [dummy_t]
    vec_specs = [("mix0", W["mix_norm_g"][0], 8), ("mix1", W["mix_norm_g"][1], 8),
                 ("ffn0", W["ffn_norm_g"][0], 8), ("ffn1", W["ffn_norm_g"][1], 8),
                 ("pleg0", W["ple_norm_g"][0], 8), ("pleg1", W["ple_norm_g"][1], 8),
                 ("kvg", W["kv_norm_g"], 8), ("fing", W["final_norm_g"], 8),
                 ("bin", W["gmlp_b_in"][0], 48), ("lng", W["gmlp_ln_g"][0], 24), ("lnb", W["gmlp_ln_b"][0], 24),
                 ("bout", W["gmlp_b_out"][0], 8), ("bo", W["attn_b_o"][0], 8)]
    V = {}
    for nm, src, nch in vec_specs:
        V[nm] = alloc([128, nch], F32)
    vstage = alloc([48, 128], F32)

    MS("dve", ident32, 0.0, ["ident32"])
    P.add("pool", lambda h: h.affine_select(out=ident32, in_=ident32, compare_op=ALU.not_equal, fill=1.0,
                                            base=0, pattern=[[-1, 128]], channel_multiplier=1),
          reads=["ident32"], writes=["ident32"])
    CP("dve", identb, ident32, ["ident32"], ["identb"])
    MS("dve", ones32, 1.0, ["ones32"])
    MS("dve", onesb, 1.0, ["onesb"])
    MS("dve", epst, EPS, ["epst"])
    MS("dve", pit, math.pi, ["pit"])
    def load_vecT(dst, src2d, n, pp, key):
        DMA("sp", vstage[0:n, 0:pp], src2d, "dc", [], ["vstage"])
        TR(bank(0)[0:pp, 0:n], vstage[0:n, 0:pp], ident32[0:n, 0:n], ["vstage", "ident32"], BK(0))
        CP("dve", dst, bank(0)[0:pp, 0:n], BK(0), [key])

    for nm, src, nch in vec_specs:
        load_vecT(V[nm], src.rearrange("(c p) -> c p", p=128), nch, 128, "v_" + nm)
    state["mark"] = state["off"]

    def xk(ns, t0, w):
        return [("x", n, b) for n in ns for b in range(t0 // 128, (t0 + w) // 128)]

    ALLN = list(range(8))

    def rms_stats(t0, w, sq, rstd, bk):
        for c in range(8):
            TT("dve" if c % 2 == 0 else "pool", sq[:, c, 0:w], xT[:, c, t0:t0 + w], xT[:, c, t0:t0 + w], ALU.mult,
               xk([c], t0, w), [("sq", c)])
        for c in range(8):
            MM(bank(bk)[:, 0:w], onesb, sq[:, c, 0:w], c == 0, c == 7, ["onesb", ("sq", c)], BK(bk))
        ACT(rstd[:, 0:w], bank(bk)[:, 0:w], AF.Sqrt, BK(bk) + ["epst"], ["rstd"], bias=epst, scale=1.0 / D)
        RECIP(rstd[:, 0:w], rstd[:, 0:w], ["rstd"], ["rstd"])

    def rms_apply(t0, w, rstd, gain, dst, dkey, engs=("dve",)):
        for c in range(8):
            STT(engs[c % len(engs)], dst[:, c, 0:w], xT[:, c, t0:t0 + w], gain[:, c:c + 1], rstd[:, 0:w],
                ALU.mult, ALU.mult, xk([c], t0, w) + ["rstd"], [(dkey, c)])

    class NormPipe:
        def __init__(self, tiles, gain, hTs, sq, rstd_of, bk=7):
            self.tiles, self.gain, self.hTs, self.sq, self.rstd_of, self.bk = tiles, gain, hTs, sq, rstd_of, bk

        def early(self, i):
            if i >= len(self.tiles):
                return
            t0, w = self.tiles[i]
            for c in range(8):
                TT("dve" if c % 2 == 0 else "pool", self.sq[:, c, 0:w], xT[:, c, t0:t0 + w], xT[:, c, t0:t0 + w],
                   ALU.mult, xk([c], t0, w), [("sq", c)])

        def late(self, i):
            if i >= len(self.tiles):
                return
            t0, w = self.tiles[i]
            bk = self.bk
            rstd = self.rstd_of(i)
            rk = ("rstd", i % 2)
            for c in range(8):
                MM(bank(bk)[:, 0:w], onesb, self.sq[:, c, 0:w], c == 0, c == 7, ["onesb", ("sq", c)], BK(bk))
            ACT(rstd, bank(bk)[:, 0:w], AF.Sqrt, BK(bk) + ["epst"], [rk], bias=epst, scale=1.0 / D)
            RECIP(rstd, rstd, [rk], [rk])
            dst = self.hTs[i % 2]
            for c in range(8):
                STT("dve", dst[:, c, 0:w], xT[:, c, t0:t0 + w], self.gain[:, c:c + 1], rstd,
                    ALU.mult, ALU.mult, xk([c], t0, w) + [rk], [("h", i % 2, c)])

    class Stream:
        def __init__(self, prefix, nslots, shape, srcs):
            self.slots = [alloc(shape, BF16) for _ in range(nslots)]
            self.prefix = prefix
            self.n = nslots
            self.srcs = srcs
            self.issued = 0
            self.cur = 0

        def _issue(self):
            i = self.issued
            slot = self.slots[i % self.n]
            src = self.srcs[i]
            sem = "%s%d" % (self.prefix, i % self.n)
            key = (self.prefix, i % self.n)
            if isinstance(src, tuple) and src[0] == "r":
                DMA("sp", slot.rearrange("p a b -> p (a b)"), src[1], "h" + sem, [src[2]], [key])
            elif isinstance(src, tuple) and src[0] == "w":
                DMA("pool", slot, src[1], sem, [], [key])
                DMA("sp", src[2], slot.rearrange("p a b -> p (a b)"), "x" + sem, [key], [src[3]])
            else:
                dst = slot
                if len(src.shape) == 3 and tuple(src.shape) != tuple(slot.shape):
                    dst = slot[:, 0:src.shape[1], 0:src.shape[2]]
                DMA("pool", dst, src, sem, [], [key])
            self.issued += 1

        def next(self):
            while self.issued < len(self.srcs) and self.issued < self.cur + self.n:
                self._issue()
            i = self.cur
            self.cur += 1
            return self.slots[i % self.n], (self.prefix, i % self.n)

    xin = [alloc([128, D], F32) for _ in range(2)]
    for b in range(NF // 128):
        xi = xin[b % 2]
        DMA("sp", xi, x_in[b * 128:(b + 1) * 128, :], "dx" if b % 2 == 0 else "dx2", [], [("xin", b % 2)])
        for half in range(2):
            bk = 4 * (b % 2) + 2 * half
            for c in range(4):
                cc = half * 4 + c
                TR(bank(bk)[:, c * 128:(c + 1) * 128], xi[:, cc * 128:(cc + 1) * 128], ident32,
                   [("xin", b % 2), "ident32"], BK(bk))
            CP("act" if half == 0 else "dve", xT[:, half * 4:half * 4 + 4, b * 128:(b + 1) * 128],
               bank(bk).rearrange("p (c t) -> p c t", c=4), BK(bk), xk(range(half * 4, half * 4 + 4), b * 128, 128))
    epoch()

    tiles0 = [(0, 128)] + [(128 + 512 * i, 512) for i in range(4)]

    tiles1 = [(128, 512), (0, 128)] + [(128 + 512 * i, 512) for i in range(1, 4)]
    if stop_after >= 1:
        W1 = 512
        hTs = [alloc([128, 8, W1], BF16) for _ in range(2)]
        sq = alloc([128, 8, W1], BF16)
        rstds = [alloc([128, W1], F32) for _ in range(2)]
        uT = alloc([128, 24, W1], BF16)
        vgs = [alloc([128, GH], BF16) for _ in range(4)]
        sjunk = alloc([128, 512], BF16)
        Ct = alloc([128, 24, 128], F32)
        tmpm_off = state["off"]
        tmpm = [alloc([128, 4, 128], F32) for _ in range(2)]
        wsTb = alloc([128, 8, 128], BF16)
        binv = alloc([1, GH], BF16)
        onesrow = alloc([1, 128], BF16)
        st1s = [alloc([128, 8], F32) for _ in range(4)]
        ssum = alloc([128, 4, 16], F32)
        w_in = W["gmlp_w_in"][0].rearrange("(k p) n -> p k n", p=128)
        w_out = W["gmlp_w_out"][0].rearrange("(m p) n -> p m n", p=128)
        scrA = nc.dram_tensor("scrA", [12, 128, 8 * 512], BF16).ap()
        scrC = nc.dram_tensor("scrC", [12, 128, 2 * 1024], BF16).ap()
        srcA, srcC = [], []
        for ti_, _ in enumerate(tiles1):
            for blk in (6, 7, 8, 9, 10, 11, 0, 1, 2, 3, 4, 5):
                col = blk * 512 if blk < 6 else GH + (blk - 6) * 512
                if ti_ == 0:
                    srcA.append(("w", w_in[:, :, col:col + 512], scrA[blk], ("scrA", blk)))
                else:
                    srcA.append(("r", scrA[blk], ("scrA", blk)))
            for mb in range(12):
                if ti_ == 0:
                    srcC.append(("w", w_out[:, mb * 2:(mb + 1) * 2, :], scrC[mb], ("scrC", mb)))
                else:
                    srcC.append(("r", scrC[mb], ("scrC", mb)))
        stA = Stream("wa", 3, [128, 8, 512], srcA)
        stC = Stream("wc", 2, [128, 2, 1024], srcC)
        off_save = state["off"]
        state["off"] = tmpm_off
        wsl = alloc([128, 128], F32)
        wsT32 = alloc([128, 128], F32)
        rsb = alloc([128, 128], F32)
        bsb = alloc([128, 128], F32)
        state["off"] = off_save
        MS("dve", onesrow, 1.0, ["onesrow"])
        DMA("pool", binv, W["gmlp_b_in"][0:1, GH:GF], "wb0", [], ["binv"])
        for g in range(8):
            DMA("sp", wsl, W["gmlp_w_s"][0, g], "dp", [], ["wsl"])
            P.add("pool", lambda h: h.affine_select(out=wsl, in_=wsl, compare_op=ALU.is_ge, fill=0.0, base=0,
                                                    pattern=[[-1, 128]], channel_multiplier=1),
                  reads=["wsl"], writes=["wsl"])
            TR(bank(0)[:, 0:128], wsl, ident32, ["wsl", "ident32"], BK(0))
            CP("act", wsT32, bank(0)[:, 0:128], BK(0), ["wsT32"])
            CP("dve", wsTb[:, g, :], wsT32, ["wsT32"], [("wsTb", g)])
            MM(bank(1)[:, 0:128], ones32, wsT32, True, True, ["ones32", "wsT32"], BK(1))
            CP("act", rsb, bank(1)[:, 0:128], BK(1), ["rsb"])
            DMA("sp", bsb, W["gmlp_b_s"][0, g].partition_broadcast(128), "dx", [], ["bsb"])
            for mm_ in range(3):
                m = g * 3 + mm_
                STT("dve", Ct[:, m, :], rsb, V["lnb"][:, m:m + 1], bsb, ALU.mult, ALU.add,
                    ["rsb", "bsb", "v_lnb"], [("C", m)])

        P.barrier(lambda h: h.memset(dummy_t, 0.0))
        npipe1 = NormPipe(tiles1, V["mix0"], hTs, sq, lambda i: rstds[i % 2][:, 0:tiles1[i][1]])
        npipe1.early(0)
        npipe1.late(0)
        for ti_, (t0, w) in enumerate(tiles1):
            nj = w // 128
            hT = hTs[ti_ % 2]
            hp = ti_ % 2
            npipe1.early(ti_ + 1)
            MS("dve", ssum, 0.0, ["ssum"])
            for blk in range(6):
                slot, key = stA.next()
                for j in range(nj):
                    bk = 4 + (blk * nj + j) % 4
                    for k in range(8):
                        MM(bank(bk), hT[:, k, j * 128:(j + 1) * 128], slot[:, k, :], k == 0, False,
                           [key, ("h", hp, k)], BK(bk))
                    MM(bank(bk), onesrow, binv[:, blk * 512:(blk + 1) * 512], False, True, ["onesrow", "binv"], BK(bk))
                    vblk = vgs[j][:, blk * 512:(blk + 1) * 512]
                    ACT(vblk, bank(bk), AF.Gelu_apprx_tanh, BK(bk) + ["ssum"], [("vg", j, blk)],
                        accum=ssum[:, j, blk:blk + 1])
                    ACT(sjunk, vblk, AF.Square, [("vg", j, blk), "ssum"], ["sjunk", ("vq", j, blk)],
                        accum=ssum[:, j, 8 + blk:9 + blk])
            for j in range(nj):
                vgk = [("vg", j, blk) for blk in range(6)]
                vqk = [("vq", j, blk) for blk in range(6)]
                st1 = st1s[j]
                sk = ("st1", j)
                P.add("dve", lambda h, j=j, st1=st1: h.tensor_reduce(out=st1[:, 0:1], in_=ssum[:, j, 0:6], axis=AX.X, op=ALU.add),
                      reads=vgk + ["ssum"], writes=[sk])
                P.add("dve", lambda h, j=j, st1=st1: h.tensor_reduce(out=st1[:, 6:7], in_=ssum[:, j, 8:14], axis=AX.X, op=ALU.add),
                      reads=vqk + ["ssum"], writes=[sk])
                TS("dve", st1[:, 0:1], st1[:, 0:1], 1.0 / GH, ALU.mult, [sk], [sk])
                TT("dve", st1[:, 1:2], st1[:, 0:1], st1[:, 0:1], ALU.mult, [sk], [sk])
                STT("dve", st1[:, 2:3], st1[:, 6:7], 1.0 / GH, st1[:, 1:2], ALU.mult, ALU.subtract, [sk], [sk])
                ACT(st1[:, 3:4], st1[:, 2:3], AF.Sqrt, [sk, "epst"], [sk], bias=epst)
                RECIP(st1[:, 4:5], st1[:, 3:4], [sk], [sk])
                STT("dve", st1[:, 5:6], st1[:, 0:1], -1.0, st1[:, 4:5], ALU.mult, ALU.mult, [sk], [sk])
                ACT(vgs[j], vgs[j], AF.Identity, vgk + [sk], [("vn", j)], bias=st1[:, 5:6], scale=st1[:, 4:5])
            for blk in range(6):
                if blk == 3:
                    npipe1.late(ti_ + 1)
                slot, key = stA.next()
                for mi in range(4):
                    m = blk * 4 + mi
                    bk = m % 4
                    for k in range(8):
                        MM(bank(bk)[:, 0:w], slot[:, k, mi * 128:(mi + 1) * 128], hT[:, k, 0:w], k == 0, k == 7,
                           [key, ("h", hp, k)], BK(bk))
                    ACT(uT[:, m, 0:w], bank(bk)[:, 0:w], AF.Gelu_apprx_tanh, BK(bk) + ["v_bin"], [("u", m)],
                        bias=V["bin"][:, m:m + 1])
            for j in range(nj):
                vnj = vgs[j]
                vnk = [("vn", j)] + [("vg", j, blk) for blk in range(6)]
                for mg in range(6):
                    bk = (j * 6 + mg) % 4
                    for mi in range(4):
                        m = mg * 4 + mi
                        g = m // 3
                        MM(bank(bk)[:, mi * 128:(mi + 1) * 128], vnj[:, m * 128:(m + 1) * 128], wsTb[:, g, :], True, True,
                           vnk + [("wsTb", g)], BK(bk))
                    tmi = (j * 6 + mg) % 2
                    tm = tmpm[tmi]
                    for mi in range(4):
                        m = mg * 4 + mi
                        STT("dve", tm[:, mi, :], bank(bk)[:, mi * 128:(mi + 1) * 128], V["lng"][:, m:m + 1], Ct[:, m, :],
                            ALU.mult, ALU.add, BK(bk) + [("C", m), "v_lng"], [("tm", tmi, mi)])
                    uk = [("u", mg * 4 + mi) for mi in range(4)]
                    usl = uT[:, mg * 4:mg * 4 + 4, j * 128:(j + 1) * 128]
                    TT("dve" if mg % 2 == 0 else "pool", usl, usl, tm, ALU.mult,
                       uk + [("tm", tmi, mi) for mi in range(4)], uk)
            for mb in range(12):
                slot, key = stC.next()
                for mi in range(2):
                    m = mb * 2 + mi
                    for n in range(8):
                        MM(bank(n)[:, 0:w], slot[:, mi, n * 128:(n + 1) * 128], uT[:, m, 0:w], m == 0, m == 23,
                           [key, ("u", m)], BK(n))
            for n in range(8):
                STT("dve", xT[:, n, t0:t0 + w], bank(n)[:, 0:w], V["bout"][:, n:n + 1], xT[:, n, t0:t0 + w],
                    ALU.add, ALU.add, BK(n) + xk([n], t0, w) + ["v_bout"], xk([n], t0, w))
        epoch()

    if stop_after >= 2:
        hTs = [alloc([128, 8, 512], BF16) for _ in range(2)]
        sq = alloc([128, 8, 512], BF16)
        rstds = [alloc([128, 512], F32) for _ in range(2)]
        actT = alloc([128, 22, 512], BF16)
        sg = [alloc([128, 512], F32) for _ in range(2)]
        wg = W["ffn_w_gate"][0].rearrange("(k p) n -> p k n", p=128)
        wu = W["ffn_w_up"][0].rearrange("(k p) n -> p k n", p=128)
        wd = W["ffn_w_down"][0].rearrange("(m p) n -> p m n", p=128)
        tiles2 = tiles1
        scrG = nc.dram_tensor("scrG", [11, 128, 8 * 256], BF16).ap()
        scrU = nc.dram_tensor("scrU", [11, 128, 8 * 256], BF16).ap()
        scrD = nc.dram_tensor("scrD", [11, 128, 2 * 1024], BF16).ap()
        srcA, srcB, srcC = [], [], []
        for ti_, _ in enumerate(tiles2):
            for blk in range(11):
                if ti_ == 0:
                    srcA.append(("w", wg[:, :, blk * 256:(blk + 1) * 256], scrG[blk], ("scrG", blk)))
                    srcB.append(("w", wu[:, :, blk * 256:(blk + 1) * 256], scrU[blk], ("scrU", blk)))
                else:
                    srcA.append(("r", scrG[blk], ("scrG", blk)))
                    srcB.append(("r", scrU[blk], ("scrU", blk)))
            for blk in range(11):
                if ti_ == 0:
                    srcC.append(("w", wd[:, blk * 2:(blk + 1) * 2, :], scrD[blk], ("scrD", blk)))
                else:
                    srcC.append(("r", scrD[blk], ("scrD", blk)))
        stA = Stream("wa", 3, [128, 8, 256], srcA)
        stB = Stream("wb", 3, [128, 8, 256], srcB)
        stC = Stream("wc", 3, [128, 2, 1024], srcC)
        npipe = NormPipe(tiles2, V["ffn0"], hTs, sq, lambda i: rstds[i % 2][:, 0:tiles2[i][1]])
        npipe.early(0)
        npipe.late(0)
        for ti_, (t0, w) in enumerate(tiles2):
            hT = hTs[ti_ % 2]
            hp = ti_ % 2
            npipe.early(ti_ + 1)
            for blk in range(11):
                if blk == 8:
                    npipe.late(ti_ + 1)
                sa, ka = stA.next()
                sb_, kb = stB.next()
                for fi in range(2):
                    f = blk * 2 + fi
                    bg = (f % 3) * 2
                    bu = bg + 1
                    for k in range(8):
                        MM(bank(bg)[:, 0:w], sa[:, k, fi * 128:(fi + 1) * 128], hT[:, k, 0:w], k == 0, k == 7,
                           [ka, ("h", hp, k)], BK(bg))
                    for k in range(8):
                        MM(bank(bu)[:, 0:w], sb_[:, k, fi * 128:(fi + 1) * 128], hT[:, k, 0:w], k == 0, k == 7,
                           [kb, ("h", hp, k)], BK(bu))
                    s = sg[f % 2]
                    ACT(s[:, 0:w], bank(bg)[:, 0:w], AF.Silu, BK(bg), [("sg", f % 2)])
                    TT("dve", actT[:, f, 0:w], s[:, 0:w], bank(bu)[:, 0:w], ALU.mult, [("sg", f % 2)] + BK(bu), [("act", f)])
            for mb in range(11):
                slot, key = stC.next()
                for mi in range(2):
                    f = mb * 2 + mi
                    for n in range(8):
                        MM(bank(n)[:, 0:w], slot[:, mi, n * 128:(n + 1) * 128], actT[:, f, 0:w], f == 0, f == 21,
                           [key, ("act", f)], BK(n))
            for n in range(8):
                TT("dve", xT[:, n, t0:t0 + w], bank(n)[:, 0:w], xT[:, n, t0:t0 + w], ALU.add,
                   BK(n) + xk([n], t0, w), xk([n], t0, w))
        epoch()

    def ple_stage(layer, p_src, tiles, p_off, gain):
        hTs = [alloc([128, 8, 512], BF16) for _ in range(2)]
        sq = alloc([128, 8, 512], BF16)
        rstds = [alloc([128, 512], F32) for _ in range(2)]
        wgt = alloc([128, 8, D], BF16)
        wpj = alloc([128, 2, D], BF16)
        pin = [alloc([128, PLE], F32) for _ in range(2)]
        pT = alloc([128, 2, 512], BF16)
        gs = [alloc([128, 512], F32) for _ in range(2)]
        tm = [alloc([128, 512], F32) for _ in range(2)]
        DMA("pool", wgt, W["ple_w_gate"][layer].rearrange("(k p) n -> p k n", p=128), "wa0", [], ["wgt"])
        DMA("pool", wpj, W["ple_w_proj"][layer].rearrange("(k p) n -> p k n", p=128), "wa1", [], ["wpj"])
        npipe = NormPipe(tiles, gain, hTs, sq, lambda i: rstds[i % 2][:, 0:tiles[i][1]])
        npipe.early(0)
        npipe.late(0)
        for ti_, (t0, w) in enumerate(tiles):
            hT = hTs[ti_ % 2]
            hp = ti_ % 2
            npipe.early(ti_ + 1)
            for j in range(w // 128):
                pi = pin[j % 2]
                r0 = t0 - p_off + j * 128
                DMA("sp", pi, p_src[r0:r0 + 128, :], "dp" if j % 2 == 0 else "dx", [], [("pin", j % 2)])
                for c in range(2):
                    TR(bank(6)[:, c * 128:(c + 1) * 128], pi[:, c * 128:(c + 1) * 128], ident32,
                       [("pin", j % 2), "ident32"], BK(6))
                CP("act", pT[:, :, j * 128:(j + 1) * 128], bank(6)[:, 0:256].rearrange("p (c t) -> p c t", c=2),
                   BK(6), [("pT", j)])
            PK = [("pT", j) for j in range(w // 128)]
            for n in range(8):
                if n == 4:
                    npipe.late(ti_ + 1)
                bg = (n % 3) * 2
                bp = bg + 1
                for k in range(8):
                    MM(bank(bg)[:, 0:w], wgt[:, k, n * 128:(n + 1) * 128], hT[:, k, 0:w], k == 0, k == 7,
                       ["wgt", ("h", hp, k)], BK(bg))
                for k in range(2):
                    MM(bank(bp)[:, 0:w], wpj[:, k, n * 128:(n + 1) * 128], pT[:, k, 0:w], k == 0, k == 1,
                       ["wpj"] + PK, BK(bp))
                g_ = gs[n % 2]
                t_ = tm[n % 2]
                ACT(g_[:, 0:w], bank(bg)[:, 0:w], AF.Sigmoid, BK(bg), [("gs", n % 2)])
                TT("dve", t_[:, 0:w], g_[:, 0:w], bank(bp)[:, 0:w], ALU.mult, [("gs", n % 2)] + BK(bp), [("tm", n % 2)])
                TT("pool", xT[:, n, t0:t0 + w], t_[:, 0:w], xT[:, n, t0:t0 + w], ALU.add,
                   [("tm", n % 2)] + xk([n], t0, w), xk([n], t0, w))
        epoch()

    if stop_after >= 3:
        ple_stage(0, p0_in, tiles0, 0, V["pleg0"])

    tiles_own = [(128 + 512 * i, 512) for i in range(4)]

    if stop_after >= 4:
        kT = alloc([64, 4, NF], BF16)
        Vt = alloc([128, NF // 128, 256], BF16)
        cosT = alloc([64, NF], F32)
        sinT = alloc([64, NF], F32)
        rstd_all = alloc([128, NF], F32)
        mark_save = state["mark"]
        state["mark"] = state["off"]
        posb = alloc([64, NF], F32)
        ang = alloc([64, NF], F32)
        kf = alloc([64, NF], F32)
        ki = alloc([64, NF], I32)
        fi32 = alloc([64, 1], I32)
        fq = alloc([64, 1], F32)
        sgn = alloc([64, 1], F32)
        DMA("sp", posb, pos_in.partition_broadcast(64), "dp", [], ["posb"])
        P.add("pool", lambda h: h.iota(fi32, pattern=[[0, 1]], base=0, channel_multiplier=1), writes=["fi32"])
        CP("dve", fq, fi32, ["fi32"], ["fq"])
        TS("dve", fq[32:64], fq[32:64], -32.0, ALU.add, ["fq"], ["fq"])
        ACT(fq, fq, AF.Exp, ["fq"], ["fq"], scale=-math.log(10000.0) / 32.0)
        MS("dve", sgn[0:32], -1.0, ["sgn"])
        MS("dve", sgn[32:64], 1.0, ["sgn"])
        TWO_PI = 2.0 * math.pi

        def sin_table(dst, shift, key):
            TS("dve", ang, posb, fq[:, 0:1], ALU.mult, ["posb", "fq"], ["ang"], s2=shift, op1=ALU.add)
            TS("dve", ki, ang, 1.0 / TWO_PI, ALU.mult, ["ang"], ["ki"])
            CP("dve", kf, ki, ["ki"], ["kf"])
            STT("dve", ang, kf, -TWO_PI, ang, ALU.mult, ALU.add, ["kf", "ang"], ["ang"])
            TS("dve", kf, ang, math.pi, ALU.is_gt, ["ang"], ["kf"], s2=-TWO_PI, op1=ALU.mult)
            TT("dve", ang, ang, kf, ALU.add, ["ang", "kf"], ["ang"])
            TS("dve", kf, ang, -math.pi, ALU.is_lt, ["ang"], ["kf"], s2=TWO_PI, op1=ALU.mult)
            TT("dve", ang, ang, kf, ALU.add, ["ang", "kf"], ["ang"])
            ACT(dst, ang, AF.Sin, ["ang"], [key])

        sin_table(cosT, math.pi / 2.0, "cosT")
        sin_table(sinT, 0.0, "sinT")
        TS("dve", sinT, sinT, sgn[:, 0:1], ALU.mult, ["sinT", "sgn"], ["sinT"])
        epoch_keep = True
        hTs = [alloc([128, 8, 512], BF16) for _ in range(2)]
        sq = alloc([128, 8, 512], BF16)
        wk = alloc([128, 8, 256], BF16)
        wks = alloc([128, 8, 256], BF16)
        wv = alloc([128, 8, 256], BF16)
        bk_t = alloc([64, 4], F32)
        bks_t = alloc([64, 4], F32)
        bvb = alloc([128, 256], F32)
        ra = [alloc([64, 512], F32) for _ in range(2)]
        rb = [alloc([64, 512], F32) for _ in range(2)]
        wkv_v = W["w_kv"].rearrange("(k p) n -> p k n", p=128)
        DMA("pool", wk, wkv_v[:, :, 0:256], "wa0", [], ["wk"])
        DMA("pool", wv, wkv_v[:, :, 256:512], "wa1", [], ["wv"])
        wk_v = wk.rearrange("p k (g two j) -> p k g two j", two=2, j=32)
        wks_v = wks.rearrange("p k (g two j) -> p k g two j", two=2, j=32)
        for k in range(8):
            CP("pool", wks_v[:, k, :, 0, :], wk_v[:, k, :, 1, :], ["wk"], [("wks", k, 0)])
            CP("pool", wks_v[:, k, :, 1, :], wk_v[:, k, :, 0, :], ["wk"], [("wks", k, 1)])
        bkv = W["b_kv"]
        load_vecT(bk_t, bkv[0:256].rearrange("(g p) -> g p", p=64), 4, 64, "bk_t")
        bkh = bkv[0:256].rearrange("(g two j) -> g two j", two=2, j=32)
        DMA("sp", vstage[0:4, 0:32], bkh[:, 1, :], "dc", [], ["vstage"])
        DMA("sp", vstage[0:4, 32:64], bkh[:, 0, :], "dc", [], ["vstage"])
        TR(bank(0)[0:64, 0:4], vstage[0:4, 0:64], ident32[0:4, 0:4], ["vstage", "ident32"], BK(0))
        CP("dve", bks_t, bank(0)[0:64, 0:4], BK(0), ["bks_t"])
        DMA("sp", bvb, bkv[256:512].partition_broadcast(128), "dc", [], ["bvb"])
        npipe = NormPipe(tiles0, V["kvg"], hTs, sq, lambda i: rstd_all[:, tiles0[i][0]:tiles0[i][0] + tiles0[i][1]])
        npipe.early(0)
        npipe.late(0)
        for ti_, (t0, w) in enumerate(tiles0):
            hT = hTs[ti_ % 2]
            hp = ti_ % 2
            npipe.early(ti_ + 1)
            for g in range(4):
                if g == 2:
                    npipe.late(ti_ + 1)
                b0 = (g % 2) * 2
                for k in range(8):
                    MM(bank(b0)[0:64, 0:w], wk[:, k, g * 64:(g + 1) * 64], hT[:, k, 0:w], k == 0, k == 7,
                       ["wk", ("h", hp, k)], BK(b0))
                for k in range(8):
                    MM(bank(b0 + 1)[0:64, 0:w], wks[:, k, g * 64:(g + 1) * 64], hT[:, k, 0:w], k == 0, k == 7,
                       [("wks", k, 0), ("wks", k, 1), ("h", hp, k)], BK(b0 + 1))
                a_ = ra[g % 2]
                b_ = rb[g % 2]
                STT("dve", a_[:, 0:w], bank(b0)[0:64, 0:w], bk_t[:, g:g + 1], cosT[:, t0:t0 + w], ALU.add, ALU.mult,
                    BK(b0) + ["bk_t", "cosT"], [("ra", g % 2)])
                STT("dve", b_[:, 0:w], bank(b0 + 1)[0:64, 0:w], bks_t[:, g:g + 1], sinT[:, t0:t0 + w], ALU.add, ALU.mult,
                    BK(b0 + 1) + ["bks_t", "sinT"], [("rb", g % 2)])
                TT("pool", kT[:, g, t0:t0 + w], a_[:, 0:w], b_[:, 0:w], ALU.add, [("ra", g % 2), ("rb", g % 2)],
                   [("kT", g, b) for b in range(t0 // 128, (t0 + w) // 128)])
            for j in range(w // 128):
                blk = t0 // 128 + j
                bkk = 4 + j % 2
                for k in range(8):
                    MM(bank(bkk)[:, 0:256], hT[:, k, j * 128:(j + 1) * 128], wv[:, k, :], k == 0, k == 7,
                       ["wv", ("h", hp, k)], BK(bkk))
                TT("dve", Vt[:, blk, :], bank(bkk)[:, 0:256], bvb, ALU.add, BK(bkk) + ["bvb"], [("V", blk)])
        epoch()
        WQ = 256
        hT = alloc([128, 8, WQ], BF16)
        wq = alloc([128, 8, D], BF16)
        wqs = alloc([128, 8, D], BF16)
        wo = alloc([128, 8, D], BF16)
        bq_t = alloc([64, 16], F32)
        bqs_t = alloc([64, 16], F32)
        sinkb = alloc([128, 16], F32)
        qT = alloc([64, 16, WQ], BF16)
        attnT = alloc([128, 8, WQ], BF16)
        ra = [alloc([64, WQ], F32)] * 2
        rb = [alloc([64, WQ], F32)] * 2
        maskt = alloc([128, 256], F32)
        mask0 = alloc([128, 256], F32)
        hmt = alloc([128, 128], F32)
        sms = [alloc([128, 4, 256], F32) for _ in range(2)]
        Pb = alloc([128, 4, 256], BF16)
        PTs = alloc([128, 8, 128], BF16)
        mxs = [alloc([128, 4], F32) for _ in range(2)]
        nmxs = [alloc([128, 4], F32) for _ in range(2)]
        rs_all = [alloc([128, 16], F32) for _ in range(2)]
        es_all = [alloc([128, 16], F32) for _ in range(2)]
        rinv = alloc([128, 16], F32)
        ao = alloc([128, 16, 64], BF16)
        wq_v = W["attn_w_q"][0].rearrange("(k p) n -> p k n", p=128)
        DMA("pool", wq, wq_v, "wa0", [], ["wq"])
        DMA("pool", wo, W["attn_w_o"][0].rearrange("(k p) n -> p k n", p=128), "wa1", [], ["wo"])
        wqv = wq.rearrange("p k (g two j) -> p k g two j", two=2, j=32)
        wqs_v = wqs.rearrange("p k (g two j) -> p k g two j", two=2, j=32)
        for k in range(8):
            CP("pool", wqs_v[:, k, :, 0, :], wqv[:, k, :, 1, :], ["wq"], [("wqs", k, 0)])
            CP("pool", wqs_v[:, k, :, 1, :], wqv[:, k, :, 0, :], ["wq"], [("wqs", k, 1)])
        bq = W["attn_b_q"][0]
        load_vecT(bq_t, bq.rearrange("(g p) -> g p", p=64), 16, 64, "bq_t")
        bqh = bq.rearrange("(g two j) -> g two j", two=2, j=32)
        DMA("sp", vstage[0:16, 0:32], bqh[:, 1, :], "dc", [], ["vstage"])
        DMA("sp", vstage[0:16, 32:64], bqh[:, 0, :], "dc", [], ["vstage"])
        TR(bank(0)[0:64, 0:16], vstage[0:16, 0:64], ident32[0:16, 0:16], ["vstage", "ident32"], BK(0))
        CP("dve", bqs_t, bank(0)[0:64, 0:16], BK(0), ["bqs_t"])
        DMA("sp", sinkb, W["attn_sinks"][0].partition_broadcast(128), "dc", [], ["sinkb"])
        DMA("sp", hmt, hm_in, "dc", [], ["hmt"])
        MS("dve", maskt, 0.0, ["maskt"])
        P.add("pool", lambda h: h.affine_select(out=maskt, in_=maskt, compare_op=ALU.is_ge, fill=NEG, base=-1,
                                                pattern=[[1, 256]], channel_multiplier=-1),
              reads=["maskt"], writes=["maskt"])
        P.add("pool", lambda h: h.affine_select(out=maskt, in_=maskt, compare_op=ALU.is_ge, fill=NEG, base=128,
                                                pattern=[[-1, 256]], channel_multiplier=1),
              reads=["maskt"], writes=["maskt"])
        CP("dve", mask0, maskt, ["maskt"], ["mask0"])
        TT("dve", mask0[:, 0:128], mask0[:, 0:128], hmt, ALU.add, ["mask0", "hmt"], ["mask0"])

        for ti in range(OWN // WQ):
            t0 = 128 + ti * WQ
            w = WQ
            rms_apply(t0, w, rstd_all[:, t0:t0 + w], V["mix1"], hT, "h")
            for hd in range(16):
                b0 = (hd % 2) * 2
                for k in range(8):
                    MM(bank(b0)[0:64, 0:w], wq[:, k, hd * 64:(hd + 1) * 64], hT[:, k, 0:w], k == 0, k == 7,
                       ["wq", ("h", k)], BK(b0))
                for k in range(8):
                    MM(bank(b0 + 1)[0:64, 0:w], wqs[:, k, hd * 64:(hd + 1) * 64], hT[:, k, 0:w], k == 0, k == 7,
                       [("wqs", k, 0), ("wqs", k, 1), ("h", k)], BK(b0 + 1))
                a_ = ra[0]
                b_ = rb[0]
                STT("dve", a_, bank(b0)[0:64, 0:w], bq_t[:, hd:hd + 1], cosT[:, t0:t0 + w], ALU.add, ALU.mult,
                    BK(b0) + ["bq_t", "cosT"], [("ra", 0)])
                STT("dve", b_, bank(b0 + 1)[0:64, 0:w], bqs_t[:, hd:hd + 1], sinT[:, t0:t0 + w], ALU.add, ALU.mult,
                    BK(b0 + 1) + ["bqs_t", "sinT"], [("rb", 0)])
                TT("pool", qT[:, hd, :], a_, b_, ALU.add, [("ra", 0), ("rb", 0)], [("q", hd)])
            iters = [(jb, g) for jb in range(w // 128) for g in range(4)]

            def phaseA(i):
                jb, g = iters[i]
                fb = t0 // 128 + jb
                mk = mask0 if fb == 1 else maskt
                mkey = "mask0" if fb == 1 else "maskt"
                par = i % 2
                bp = jb % 2
                sb0 = par * 2
                Sv = PS[:, 512 * sb0:512 * sb0 + 1024].rearrange("p (a b) -> p a b", a=4)
                for hh in range(4):
                    hd = 4 * g + hh
                    MM(PS[:, 512 * sb0 + hh * 256:512 * sb0 + (hh + 1) * 256], qT[:, hd, jb * 128:(jb + 1) * 128],
                       kT[:, g, (fb - 1) * 128:(fb + 1) * 128], True, True,
                       [("q", hd), ("kT", g, fb - 1), ("kT", g, fb)], BK(sb0, 2))
                STT("dve", sms[par], Sv, 0.125, mk.unsqueeze(1).to_broadcast([128, 4, 256]), ALU.mult, ALU.add,
                    BK(sb0, 2) + [mkey], [("sm", par)])
                P.add("dve", lambda h: h.tensor_reduce(out=mxs[par], in_=sms[par], axis=AX.X, op=ALU.max),
                      reads=[("sm", par)], writes=[("mx", par)])
                TT("dve", mxs[par], mxs[par], sinkb[:, 4 * g:4 * g + 4], ALU.max, [("mx", par), "sinkb"], [("mx", par)])
                TS("dve", nmxs[par], mxs[par], -1.0, ALU.mult, [("mx", par)], [("nmx", par)])
                if g == 0:
                    MS("dve", rs_all[bp], 0.0, [("rs", bp)] + [("rsc", bp, c) for c in range(16)])
                TT("dve", es_all[bp][:, 4 * g:4 * g + 4], sinkb[:, 4 * g:4 * g + 4], mxs[par], ALU.subtract,
                   ["sinkb", ("mx", par)], [("es", bp, g)])

            def phaseB(i):
                jb, g = iters[i]
                par = i % 2
                bp = jb % 2
                for hh in range(4):
                    ACT(Pb[:, hh, :], sms[par][:, hh, :], AF.Exp, [("sm", par), ("nmx", par), ("rs", bp)],
                        [("P", hh), ("rsc", bp, 4 * g + hh)],
                        bias=nmxs[par][:, hh:hh + 1], accum=rs_all[bp][:, 4 * g + hh:4 * g + hh + 1])
                ACT(es_all[bp][:, 4 * g:4 * g + 4], es_all[bp][:, 4 * g:4 * g + 4], AF.Exp, [("es", bp, g)], [("es", bp, g)])

            def phaseC(i):
                jb, g = iters[i]
                fb = t0 // 128 + jb
                bp = jb % 2
                ptb = bankb(4)
                for hh in range(4):
                    for half in range(2):
                        TR(ptb[:, (hh * 2 + half) * 128:(hh * 2 + half + 1) * 128], Pb[:, hh, half * 128:(half + 1) * 128],
                           identb, [("P", hh), "identb"], BK(4))
                CP("act" if i % 2 == 0 else "dve", PTs, ptb.rearrange("p (a b) -> p a b", a=8), BK(4), ["PTs"])
                for hh in range(4):
                    hd = 4 * g + hh
                    for half in range(2):
                        MM(PS[:, 512 * 5 + hd * 64:512 * 5 + (hd + 1) * 64], PTs[:, hh * 2 + half, :],
                           Vt[:, fb - 1 + half, g * 64:(g + 1) * 64], half == 0, half == 1,
                           ["PTs", ("V", fb - 1 + half)], BK(5, 2))
                if g == 3:
                    TT("dve", es_all[bp], es_all[bp], rs_all[bp], ALU.add,
                       [("es", bp, gg) for gg in range(4)] + [("rsc", bp, c) for c in range(16)], [("es", bp, gg) for gg in range(4)])
                    RECIP(rinv, es_all[bp], [("es", bp, gg) for gg in range(4)], ["rinv"])
                    Ov = PS[:, 512 * 5:512 * 7].rearrange("p (a b) -> p a b", a=16)
                    TT("dve", ao, Ov, rinv.unsqueeze(2).to_broadcast([128, 16, 64]), ALU.mult,
                       BK(5, 2) + ["rinv"], ["ao"])
                    aof = ao.rearrange("p a b -> p (a b)")
                    atb = bankb(7)
                    for c in range(8):
                        TR(atb[:, c * 128:(c + 1) * 128], aof[:, c * 128:(c + 1) * 128], identb, ["ao", "identb"], BK(7))
                    CP("act", attnT[:, :, jb * 128:(jb + 1) * 128], atb.rearrange("p (a b) -> p a b", a=8), BK(7),
                       [("attnT", jb)])

            phaseA(0)
            for i in range(len(iters)):
                if i + 1 < len(iters):
                    phaseA(i + 1)
                phaseB(i)
                phaseC(i)
            AK = [("attnT", jb) for jb in range(w // 128)]
            for n in range(8):
                bk = n % 4
                for k in range(8):
                    MM(bank(bk)[:, 0:w], wo[:, k, n * 128:(n + 1) * 128], attnT[:, k, :], k == 0, k == 7,
                       ["wo"] + AK, BK(bk))
                STT("dve", xT[:, n, t0:t0 + w], bank(bk)[:, 0:w], V["bo"][:, n:n + 1], xT[:, n, t0:t0 + w],
                    ALU.add, ALU.add, BK(bk) + xk([n], t0, w) + ["v_bo"], xk([n], t0, w))
        state["mark"] = mark_save
        epoch()

    if stop_after >= 5:
        hn = alloc([128, 8, OWN], BF16)
        cmb = alloc([128, 16, NE], F32)
        mark_save5 = state["mark"]
        state["mark"] = state["off"]
        sq = alloc([128, 8, 512], BF16)
        rstd = alloc([128, 512], F32)
        hn32 = alloc([128, 8, 128], F32)
        wr = alloc([128, 8, NE], F32)
        lg = alloc([128, NE], F32)
        top8 = alloc([128, 8], F32)
        mk8 = alloc([128, NE], F32)
        ex8 = alloc([128, NE], F32)
        den = alloc([128, 2], F32)
        DMA("sp", wr, W["moe_w_router"][0].rearrange("(k p) e -> p k e", p=128), "dc", [], ["wr"])
        for (t0, w) in tiles_own:
            rms_stats(t0, w, sq, rstd, 7)
            o0 = t0 - 128
            for c in range(8):
                STT("dve", hn[:, c, o0:o0 + w], xT[:, c, t0:t0 + w], V["ffn1"][:, c:c + 1],
                    rstd[:, 0:w], ALU.mult, ALU.mult, xk([c], t0, w) + ["rstd"], [("hn", c, o0 // 512)])
            for j in range(4):
                jj = o0 // 128 + j
                for c in range(8):
                    STT("dve", hn32[:, c, :], xT[:, c, t0 + j * 128:t0 + (j + 1) * 128],
                        V["ffn1"][:, c:c + 1], rstd[:, j * 128:(j + 1) * 128], ALU.mult, ALU.mult,
                        xk([c], t0 + j * 128, 128) + ["rstd"], [("hn32", c)])
                for c in range(8):
                    MM(bank(6)[:, 0:NE], hn32[:, c, :], wr[:, c, :], c == 0, c == 7, [("hn32", c), "wr"], BK(6))
                CP("act", lg, bank(6)[:, 0:NE], BK(6), ["lg"])
                P.add("dve", lambda h: h.max(out=top8, in_=lg), reads=["lg"], writes=["top8"])
                TS("dve", mk8, lg, top8[:, 1:2], ALU.is_ge, ["lg", "top8"], ["mk8"])
                TS("dve", den[:, 1:2], top8[:, 0:1], -1.0, ALU.mult, ["top8"], ["den"])
                ACT(ex8, lg, AF.Exp, ["lg", "den"], ["ex8"], bias=den[:, 1:2])
                TT("dve", ex8, ex8, mk8, ALU.mult, ["ex8", "mk8"], ["ex8"])
                P.add("dve", lambda h: h.tensor_reduce(out=den[:, 0:1], in_=ex8, axis=AX.X, op=ALU.add),
                      reads=["ex8"], writes=["den"])
                RECIP(den[:, 0:1], den[:, 0:1], ["den"], ["den"])
                TS("dve", cmb[:, jj, :], ex8, den[:, 0:1], ALU.mult, ["ex8", "den"], [("cmb", jj)])
        epoch()
        cbs = [alloc([128, OWN], F32) for _ in range(2)]
        dg = [alloc([128, 128], F32) for _ in range(2)]
        sg = [alloc([128, 256], F32) for _ in range(2)]
        tg = [alloc([128, 256], F32) for _ in range(2)]
        actE = [alloc([128, 4, 256], BF16) for _ in range(2)]
        srcA, srcB, srcC = [], [], []
        for e in range(NE):
            wg = W["moe_w_gate"][0, e].rearrange("(k p) n -> p k n", p=128)
            wu = W["moe_w_up"][0, e].rearrange("(k p) n -> p k n", p=128)
            wd = W["moe_w_down"][0, e].rearrange("(m p) n -> p m n", p=128)
            for fg in range(7):
                srcA.append(wg[:, :, fg * 512:(fg + 1) * 512])
                srcB.append(wu[:, :, fg * 512:(fg + 1) * 512])
                srcC.append(wd[:, fg * 4:(fg + 1) * 4, :])
        stA = Stream("wa", 3, [128, 8, 512], srcA)
        stB = Stream("wb", 3, [128, 8, 512], srcB)
        stC = Stream("wc", 3, [128, 4, 1024], srcC)

        def emit_cb(e):
            cb = cbs[e % 2]
            for jj in range(16):
                d_ = dg[jj % 2]
                TS("dve", d_, ident32, cmb[:, jj, e:e + 1], ALU.mult, ["ident32", ("cmb", jj)], [("dg", jj % 2)])
                MM(bank(7)[:, (jj % 4) * 128:(jj % 4 + 1) * 128], ones32, d_, True, True, ["ones32", ("dg", jj % 2)], BK(7))
                if jj % 4 == 3:
                    CP("act", cb[:, (jj - 3) * 128:(jj + 1) * 128], bank(7), BK(7), [("cb", e % 2, jj // 4)])

        def emit_gu(e, sa, ka, sb_, kb, ti):
            o0 = ti * 256
            ae = actE[ti % 2]
            cb = cbs[e % 2]
            for fb in range(4):
                bk = (ti * 4 + fb) % 4
                pg = bank(bk)[:, 0:256]
                pu = bank(bk)[:, 256:512]
                for k in range(8):
                    MM(pg, sa[:, k, fb * 128:(fb + 1) * 128], hn[:, k, o0:o0 + 256], k == 0, k == 7,
                       [ka, ("hn", k, o0 // 512)], BK(bk))
                for k in range(8):
                    MM(pu, sb_[:, k, fb * 128:(fb + 1) * 128], hn[:, k, o0:o0 + 256], k == 0, k == 7,
                       [kb, ("hn", k, o0 // 512)], BK(bk))
                s_ = sg[fb % 2]
                t_ = tg[fb % 2]
                ACT(s_, pg, AF.Silu, BK(bk), [("sg", fb % 2)])
                TT("dve", t_, s_, pu, ALU.mult, [("sg", fb % 2)] + BK(bk), [("tg", fb % 2)])
                TT("pool", ae[:, fb, :], t_, cb[:, o0:o0 + 256], ALU.mult, [("tg", fb % 2), ("cb", e % 2, o0 // 512)],
                   [("ae", ti % 2, fb)])

        def emit_down(sc, kc, ti):
            o0 = ti * 256
            ae = actE[ti % 2]
            for n in range(8):
                yb = 4 + n // 2
                yv = bank(yb)[:, (n % 2) * 256:(n % 2 + 1) * 256]
                for fb in range(4):
                    MM(yv, sc[:, fb, n * 128:(n + 1) * 128], ae[:, fb, :], fb == 0, fb == 3,
                       [kc, ("ae", ti % 2, fb)], BK(yb))
            for n2 in range(4):
                yv = bank(4 + n2).rearrange("p (a b) -> p a b", a=2)
                xv = xT[:, 2 * n2:2 * n2 + 2, 128 + o0:128 + o0 + 256]
                TT("dve", xv, yv, xv, ALU.add, BK(4 + n2) + xk([2 * n2, 2 * n2 + 1], 128 + o0, 256),
                   xk([2 * n2, 2 * n2 + 1], 128 + o0, 256))

        emit_cb(0)
        for e in range(NE):
            for fg in range(7):
                sa, ka = stA.next()
                sb_, kb = stB.next()
                sc, kc = stC.next()
                for ti in range(8):
                    emit_gu(e, sa, ka, sb_, kb, ti)
                    if ti > 0:
                        emit_down(sc, kc, ti - 1)
                    if fg == 3 and ti == 3 and e + 1 < NE:
                        emit_cb(e + 1)
                emit_down(sc, kc, 7)
        state["mark"] = mark_save5
        epoch()

    if stop_after >= 6:
        ple_stage(1, p1_in, tiles_own, 128, V["pleg1"])

    gb = alloc([128, D], F32)
    DMA("sp", gb, W["final_norm_g"].partition_broadcast(128), "dc", [], ["gb"])
    yo = [alloc([128, D], F32) for _ in range(2)]
    junk = alloc([128, D], F32)
    ss = alloc([128, 4], F32)
    outs = []
    for b in range(OWN // 128):
        t0 = 128 + b * 128
        pb = 4 * (b % 2)
        for half in range(2):
            for c in range(4):
                cc = half * 4 + c
                TR(PS[:, (pb + 2 * half) * 512 + c * 128:(pb + 2 * half) * 512 + (c + 1) * 128], xT[:, cc, t0:t0 + 128], ident32,
                   xk([cc], t0, 128) + ["ident32"], BK(pb + 2 * half))
        src = [PS[:, (pb + 2 * half) * 512:(pb + 2 * half) * 512 + 512] for half in range(2)]
        y_ = yo[b % 2]
        if final_norm:
            MS("dve", ss, 0.0, ["ss"])
            for half in range(2):
                ACT(junk[:, half * 512:(half + 1) * 512], src[half], AF.Square, BK(pb + 2 * half) + ["ss"], ["junk", "ss"],
                    accum=ss[:, half:half + 1])
            TT("dve", ss[:, 2:3], ss[:, 0:1], ss[:, 1:2], ALU.add, ["ss"], ["ss"])
            ACT(ss[:, 2:3], ss[:, 2:3], AF.Sqrt, ["ss", "epst"], ["ss"], bias=epst, scale=1.0 / D)
            RECIP(ss[:, 3:4], ss[:, 2:3], ["ss"], ["ss"])
            for half in range(2):
                STT("dve", y_[:, half * 512:(half + 1) * 512], src[half], ss[:, 3:4], gb[:, half * 512:(half + 1) * 512],
                    ALU.mult, ALU.mult, BK(pb + 2 * half) + ["ss", "gb"], [("yo", b % 2)])
        else:
            for half in range(2):
                CP("dve", y_[:, half * 512:(half + 1) * 512], src[half], BK(pb + 2 * half), [("yo", b % 2)])
        outs.append(DMA("sp", y_out[b * 128:(b + 1) * 128, :], y_, "dout" if b % 2 == 0 else "dout2", [("yo", b % 2)], []))

    P.emit_all(nc, sems, final_waits=outs[-2:])
    return nc, es


_CACHE = {}


def _core_inputs(x, p, c):
    b = c // 2
    half = c % 2
    s0 = half * OWN
    xc = np.zeros((NF, D), np.float32)
    p0c = np.zeros((NF, PLE), np.float32)
    if half == 1:
        xc[:] = x[b, s0 - HALO:s0 + OWN]
        p0c[:] = p[0, b, s0 - HALO:s0 + OWN]
    else:
        xc[HALO:] = x[b, 0:OWN]
        p0c[HALO:] = p[0, b, 0:OWN]
    p1c = np.ascontiguousarray(p[1, b, s0:s0 + OWN])
    pos = np.maximum(np.arange(s0 - HALO, s0 + OWN), 0).astype(np.float32)
    hm = np.full((128, 128), 0.0 if half == 1 else NEG, np.float32)
    return {"xc": xc, "p0c": p0c, "p1c": p1c, "pos": pos, "hm": hm}


def kernel(**inputs):
    x = np.asarray(inputs["x"], np.float32)
    p = np.asarray(inputs["p"], np.float32)
    if "nc" not in _CACHE:
        _CACHE["nc"] = build()
    nc, _es = _CACHE["nc"]
    wmap = {name: np.ascontiguousarray(np.asarray(inputs[name], np.float32)) for name, _ in WEIGHT_SPECS}
    in_maps = []
    for c in range(NCORES):
        m = dict(wmap)
        m.update(_core_inputs(x, p, c))
        in_maps.append(m)
    res = run_bass_kernel_spmd(nc, in_maps, core_ids=list(range(NCORES)))
    out = np.zeros((4, SEQ, D), np.float32)
    for c in range(NCORES):
        b, half = c // 2, c % 2
        out[b, half * OWN:(half + 1) * OWN] = res.results[c]["y"]
    return out
```

```python
import math
import os
from contextlib import ExitStack

import numpy as np
import concourse.bass as bass
import concourse.mybir as mybir
from concourse.bass_utils import run_bass_kernel_spmd

F32 = mybir.dt.float32
BF16 = mybir.dt.bfloat16
I32 = mybir.dt.int32
AF = mybir.ActivationFunctionType
ALU = mybir.AluOpType
AX = mybir.AxisListType

NCORES = 8
D = 1024
SEQ = 4096
OWN = 2048
HALO = 128
NF = OWN + HALO
GF = 6144
GH = 3072
DFF = 2816
NE = 8
DFE = 3584
PLE = 256
EPS = 1e-6
NEG = -1e30

ENGS = ("pe", "act", "dve", "pool", "sp")


class Op:
    __slots__ = ("eng", "emit", "deps", "sem", "seq", "signal", "is_dma")

    def __init__(self, eng, emit, is_dma):
        self.eng = eng
        self.emit = emit
        self.deps = []
        self.sem = None
        self.seq = 0
        self.signal = False
        self.is_dma = is_dma


class Prog:
    def __init__(self):
        self.ops = {e: [] for e in ENGS}
        self.last_write = {}
        self.readers = {}
        self.dma_tot = {}
        self.epoch_op = None
        self.shared = {"dc%d" % i for i in range(8)}

    def add(self, eng, emit, reads=(), writes=(), dma_sem=None):
        op = Op(eng, emit, dma_sem is not None)
        deps = {}
        for k in reads:
            w = self.last_write.get(k)
            if w is not None:
                deps[id(w)] = w
        for k in writes:
            w = self.last_write.get(k)
            if w is not None:
                deps[id(w)] = w
            for r in self.readers.get(k, ()):
                deps[id(r)] = r
        if self.epoch_op is not None:
            deps[id(self.epoch_op)] = self.epoch_op
        for k in reads:
            self.readers.setdefault(k, []).append(op)
        for k in writes:
            self.last_write[k] = op
            self.readers[k] = []
        for d in deps.values():
            if d is op:
                continue
            if (not d.is_dma) and d.eng == eng and eng in ("pe", "sp"):
                continue
            if d.is_dma and d.sem in self.shared:
                op.deps.append((d, self.dma_tot[d.sem]))
            else:
                op.deps.append((d, None))
            d.signal = True
        if dma_sem is not None:
            op.sem = dma_sem
            self.dma_tot[dma_sem] = self.dma_tot.get(dma_sem, 0) + 16
            op.seq = self.dma_tot[dma_sem]
        self.ops[eng].append(op)
        return op

    def barrier(self, emit):
        op = Op("dve", emit, False)
        seen = {}
        for e in ENGS:
            lst = self.ops[e]
            last_nd = None
            for o in lst:
                if o.is_dma:
                    seen[id(o)] = o
                else:
                    last_nd = o
            if last_nd is not None:
                seen[id(last_nd)] = last_nd
        newest = {}
        for o in seen.values():
            if o.is_dma:
                if o.sem not in newest or newest[o.sem].seq < o.seq:
                    newest[o.sem] = o
        for o in seen.values():
            if o.is_dma and newest[o.sem] is not o:
                continue
            op.deps.append((o, None))
            o.signal = True
        self.ops["dve"].append(op)
        self.epoch_op = op
        self.last_write = {}
        self.readers = {}
        return op

    def emit_all(self, nc, sems, final_waits=()):
        for e in ENGS:
            c = 0
            for op in self.ops[e]:
                if op.is_dma:
                    continue
                if op.signal:
                    c += 1
                    op.seq = c
                    op.sem = e

        def run(e, h):
            waited = {}
            for op in self.ops[e]:
                for d, ov in op.deps:
                    v = d.seq if ov is None else ov
                    if waited.get(d.sem, 0) < v:
                        h.wait_ge(sems[d.sem], v)
                        waited[d.sem] = v
                inst = op.emit(h)
                if op.is_dma:
                    inst.then_inc(sems[op.sem], 16)
                elif op.signal:
                    inst.then_inc(sems[op.sem], 1)
            if e == "sp":
                for d in final_waits:
                    if waited.get(d.sem, 0) < d.seq:
                        h.wait_ge(sems[d.sem], d.seq)
                        waited[d.sem] = d.seq

        with nc.Block() as block:
            @block.tensor
            def _(h):
                run("pe", h)

            @block.scalar
            def _(h):
                run("act", h)

            @block.vector
            def _(h):
                run("dve", h)

            @block.gpsimd
            def _(h):
                run("pool", h)

            @block.sync
            def _(h):
                run("sp", h)


WEIGHT_SPECS = [
    ("mix_norm_g", [2, D]), ("ffn_norm_g", [2, D]),
    ("gmlp_w_in", [1, D, GF]), ("gmlp_b_in", [1, GF]), ("gmlp_ln_g", [1, GH]), ("gmlp_ln_b", [1, GH]),
    ("gmlp_w_s", [1, 8, 128, 128]), ("gmlp_b_s", [1, 8, 128]), ("gmlp_w_out", [1, GH, D]),
    ("gmlp_b_out", [1, D]), ("kv_norm_g", [D]), ("w_kv", [D, 512]), ("b_kv", [512]),
    ("attn_w_q", [1, D, D]), ("attn_b_q", [1, D]), ("attn_sinks", [1, 16]), ("attn_w_o", [1, D, D]),
    ("attn_b_o", [1, D]), ("ffn_w_gate", [1, D, DFF]), ("ffn_w_up", [1, D, DFF]),
    ("ffn_w_down", [1, DFF, D]), ("moe_w_router", [1, D, NE]), ("moe_w_gate", [1, NE, D, DFE]),
    ("moe_w_up", [1, NE, D, DFE]), ("moe_w_down", [1, NE, DFE, D]), ("ple_w_proj", [2, PLE, D]),
    ("ple_norm_g", [2, D]), ("ple_w_gate", [2, D, D]), ("final_norm_g", [D]),
]


def build(stop_after=99, final_norm=True):
    nc = bass.Bass("TRN2", target_bir_lowering=False)
    W = {}
    for name, shp in WEIGHT_SPECS:
        W[name] = nc.dram_tensor(name, shp, F32, kind="ExternalInput").ap()
    x_in = nc.dram_tensor("xc", [NF, D], F32, kind="ExternalInput").ap()
    p0_in = nc.dram_tensor("p0c", [NF, PLE], F32, kind="ExternalInput").ap()
    p1_in = nc.dram_tensor("p1c", [OWN, PLE], F32, kind="ExternalInput").ap()
    pos_in = nc.dram_tensor("pos", [NF], F32, kind="ExternalInput").ap()
    hm_in = nc.dram_tensor("hm", [128, 128], F32, kind="ExternalInput").ap()
    y_out = nc.dram_tensor("y", [OWN, D], F32, kind="ExternalOutput").ap()

    P = Prog()
    es = ExitStack()
    ARENA_B = 206 * 1024
    arena = es.enter_context(nc.sbuf_tensor("arena", [128, ARENA_B // 2], BF16))
    PS = es.enter_context(nc.psum_tensor("ps", [128, 4096], F32))
    sem_names = list(ENGS) + ["dx", "dx2", "dp", "dc", "dout", "dout2"] + ["dc%d" % i for i in range(8)] + ["wa%d" % i for i in range(4)] + \
        ["wb%d" % i for i in range(4)] + ["wc%d" % i for i in range(4)] + ["wr0", "wr1", "wr2", "wr3", "xwa0", "xwa1", "xwa2", "xwc0", "xwc1", "hwa0", "hwa1", "hwa2", "hwc0", "hwc1", "xwb0", "xwb1", "xwb2", "hwb0", "hwb1", "hwb2", "xwc2", "hwc2"]
    sems = {n: es.enter_context(nc.semaphore(n)) for n in sem_names}

    state = {"off": 0, "mark": 0}

    def alloc(shape, dt, parts=128):
        nb = int(np.prod(shape[1:])) * (4 if dt in (F32, I32) else 2)
        nb = (nb + 63) // 64 * 64
        off = state["off"]
        assert off + nb <= ARENA_B, ("arena overflow", off, nb)
        state["off"] = off + nb
        v = arena[:, off // 2:(off + nb) // 2]
        if dt in (F32, I32):
            v = v.bitcast(dt)
        n = int(np.prod(shape[1:]))
        v = v[:, 0:n]
        if len(shape) == 3:
            v = v.rearrange("p (a b) -> p a b", a=shape[1])
        elif len(shape) == 4:
            v = v.rearrange("p (a b c) -> p a b c", a=shape[1], b=shape[2])
        if shape[0] < 128:
            v = v[0:shape[0]]
        return v

    def bank(i, n=1):
        return PS[:, 512 * i:512 * (i + n)]

    def bankb(i):
        return PS[:, 512 * i:512 * (i + 1)].bitcast(BF16)

    def BK(i, n=1):
        return ["B%d" % j for j in range(i, i + n)]

    def MM(out, lhsT, rhs, start, stop, r, w):
        P.add("pe", lambda h: h.matmul(out, lhsT, rhs, start=start, stop=stop), reads=r, writes=w)

    def TR(out, in_, ident, r, w):
        P.add("pe", lambda h: h.transpose(out=out, in_=in_, identity=ident), reads=r, writes=w)

    def ACT(out, in_, func, r, w, bias=None, scale=1.0, accum=None):
        kw = {}
        if bias is not None:
            kw["bias"] = bias
        if accum is not None:
            kw["accum_out"] = accum
        P.add("act", lambda h: h.activation(out=out, in_=in_, func=func, scale=scale, **kw), reads=r, writes=w)

    def TT(eng, out, in0, in1, op, r, w):
        P.add(eng, lambda h: h.tensor_tensor(out=out, in0=in0, in1=in1, op=op), reads=r, writes=w)

    def TS(eng, out, in0, s1, op0, r, w, s2=None, op1=None):
        if op1 is None:
            P.add(eng, lambda h: h.tensor_scalar(out=out, in0=in0, scalar1=s1, scalar2=None, op0=op0), reads=r, writes=w)
        else:
            P.add(eng, lambda h: h.tensor_scalar(out=out, in0=in0, scalar1=s1, scalar2=s2, op0=op0, op1=op1), reads=r, writes=w)

    def STT(eng, out, in0, scalar, in1, op0, op1, r, w):
        P.add(eng, lambda h: h.scalar_tensor_tensor(out=out, in0=in0, scalar=scalar, in1=in1, op0=op0, op1=op1),
              reads=r, writes=w)

    def CP(eng, out, in_, r, w):
        if eng == "act":
            P.add("act", lambda h: h.copy(out=out, in_=in_), reads=r, writes=w)
        else:
            P.add(eng, lambda h: h.tensor_copy(out=out, in_=in_), reads=r, writes=w)

    def MS(eng, ap, val, w):
        P.add(eng, lambda h: h.memset(ap, val), writes=w)

    dc_rot = [0]

    def DMA(eng, out, in_, sem, r, w, slow=False):
        if sem == "dc":
            sem = "dc%d" % (dc_rot[0] % 8)
            dc_rot[0] += 1
        if slow:
            return P.add(eng, lambda h: h.dma_start(out=out, in_=in_, allow_slow_non_contiguous=True),
                         reads=r, writes=w, dma_sem=sem)
        return P.add(eng, lambda h: h.dma_start(out=out, in_=in_), reads=r, writes=w, dma_sem=sem)

    def RECIP(out, in_, r, w):
        P.add("dve", lambda h: h.reciprocal(out=out, in_=in_), reads=r, writes=w)

    def epoch():
        dummy = alloc_dummy[0]
        P.barrier(lambda h: h.memset(dummy, 0.0))
        state["off"] = state["mark"]

    xT = alloc([128, 8, NF], F32)
    ident32 = alloc([128, 128], F32)
    ones32 = alloc([128, 128], F32)
    identb = alloc([128, 128], BF16)
    onesb = alloc([128, 128], BF16)
    epst = alloc([128, 1], F32)
    pit = alloc([128, 1], F32)
    dummy_t = alloc([128, 16], F32)
    alloc_dummy = [dummy_t]
    vec_specs = [("mix0", W["mix_norm_g"][0], 8), ("mix1", W["mix_norm_g"][1], 8),
                 ("ffn0", W["ffn_norm_g"][0], 8), ("ffn1", W["ffn_norm_g"][1], 8),
                 ("pleg0", W["ple_norm_g"][0], 8), ("pleg1", W["ple_norm_g"][1], 8),
                 ("kvg", W["kv_norm_g"], 8), ("fing", W["final_norm_g"], 8),
                 ("bin", W["gmlp_b_in"][0], 48), ("lng", W["gmlp_ln_g"][0], 24), ("lnb", W["gmlp_ln_b"][0], 24),
                 ("bout", W["gmlp_b_out"][0], 8), ("bo", W["attn_b_o"][0], 8)]
    V = {}
    for nm, src, nch in vec_specs:
        V[nm] = alloc([128, nch], F32)
    vstage = alloc([48, 128], F32)

    MS("dve", ident32, 0.0, ["ident32"])
    P.add("pool", lambda h: h.affine_select(out=ident32, in_=ident32, compare_op=ALU.not_equal, fill=1.0,
                                            base=0, pattern=[[-1, 128]], channel_multiplier=1),
          reads=["ident32"], writes=["ident32"])
    CP("dve", identb, ident32, ["ident32"], ["identb"])
    MS("dve", ones32, 1.0, ["ones32"])
    MS("dve", onesb, 1.0, ["onesb"])
    MS("dve", epst, EPS, ["epst"])
    MS("dve", pit, math.pi, ["pit"])
    def load_vecT(dst, src2d, n, pp, key):
        DMA("sp", vstage[0:n, 0:pp], src2d, "dc", [], ["vstage"])
        TR(bank(0)[0:pp, 0:n], vstage[0:n, 0:pp], ident32[0:n, 0:n], ["vstage", "ident32"], BK(0))
        CP("dve", dst, bank(0)[0:pp, 0:n], BK(0), [key])

    for nm, src, nch in vec_specs:
        load_vecT(V[nm], src.rearrange("(c p) -> c p", p=128), nch, 128, "v_" + nm)
    state["mark"] = state["off"]

    def xk(ns, t0, w):
        return [("x", n, b) for n in ns for b in range(t0 // 128, (t0 + w) // 128)]

    ALLN = list(range(8))

    def rms_stats(t0, w, sq, rstd, bk):
        for c in range(8):
            TT("dve" if c % 2 == 0 else "pool", sq[:, c, 0:w], xT[:, c, t0:t0 + w], xT[:, c, t0:t0 + w], ALU.mult,
               xk([c], t0, w), [("sq", c)])
        for c in range(8):
            MM(bank(bk)[:, 0:w], onesb, sq[:, c, 0:w], c == 0, c == 7, ["onesb", ("sq", c)], BK(bk))
        ACT(rstd[:, 0:w], bank(bk)[:, 0:w], AF.Sqrt, BK(bk) + ["epst"], ["rstd"], bias=epst, scale=1.0 / D)
        RECIP(rstd[:, 0:w], rstd[:, 0:w], ["rstd"], ["rstd"])

    def rms_apply(t0, w, rstd, gain, dst, dkey, engs=("dve",)):
        for c in range(8):
            STT(engs[c % len(engs)], dst[:, c, 0:w], xT[:, c, t0:t0 + w], gain[:, c:c + 1], rstd[:, 0:w],
                ALU.mult, ALU.mult, xk([c], t0, w) + ["rstd"], [(dkey, c)])

    class NormPipe:
        def __init__(self, tiles, gain, hTs, sq, rstd_of, bk=7):
            self.tiles, self.gain, self.hTs, self.sq, self.rstd_of, self.bk = tiles, gain, hTs, sq, rstd_of, bk

        def early(self, i):
            if i >= len(self.tiles):
                return
            t0, w = self.tiles[i]
            for c in range(8):
                TT("dve" if c % 2 == 0 else "pool", self.sq[:, c, 0:w], xT[:, c, t0:t0 + w], xT[:, c, t0:t0 + w],
                   ALU.mult, xk([c], t0, w), [("sq", c)])

        def late(self, i):
            if i >= len(self.tiles):
                return
            t0, w = self.tiles[i]
            bk = self.bk
            rstd = self.rstd_of(i)
            rk = ("rstd", i % 2)
            for c in range(8):
                MM(bank(bk)[:, 0:w], onesb, self.sq[:, c, 0:w], c == 0, c == 7, ["onesb", ("sq", c)], BK(bk))
            ACT(rstd, bank(bk)[:, 0:w], AF.Sqrt, BK(bk) + ["epst"], [rk], bias=epst, scale=1.0 / D)
            RECIP(rstd, rstd, [rk], [rk])
            dst = self.hTs[i % 2]
            for c in range(8):
                STT("dve", dst[:, c, 0:w], xT[:, c, t0:t0 + w], self.gain[:, c:c + 1], rstd,
                    ALU.mult, ALU.mult, xk([c], t0, w) + [rk], [("h", i % 2, c)])

    class Stream:
        def __init__(self, prefix, nslots, shape, srcs):
            self.slots = [alloc(shape, BF16) for _ in range(nslots)]
            self.prefix = prefix
            self.n = nslots
            self.srcs = srcs
            self.issued = 0
            self.cur = 0

        def _issue(self):
            i = self.issued
            slot = self.slots[i % self.n]
            src = self.srcs[i]
            sem = "%s%d" % (self.prefix, i % self.n)
            key = (self.prefix, i % self.n)
            if isinstance(src, tuple) and src[0] == "r":
                DMA("sp", slot.rearrange("p a b -> p (a b)"), src[1], "h" + sem, [src[2]], [key])
            elif isinstance(src, tuple) and src[0] == "w":
                DMA("pool", slot, src[1], sem, [], [key])
                DMA("sp", src[2], slot.rearrange("p a b -> p (a b)"), "x" + sem, [key], [src[3]])
            else:
                dst = slot
                if len(src.shape) == 3 and tuple(src.shape) != tuple(slot.shape):
                    dst = slot[:, 0:src.shape[1], 0:src.shape[2]]
                DMA("pool", dst, src, sem, [], [key])
            self.issued += 1

        def prefetch(self):
            while self.issued < len(self.srcs) and self.issued < self.cur + self.n:
                self._issue()

        def next(self):
            while self.issued < len(self.srcs) and self.issued < self.cur + self.n:
                self._issue()
            i = self.cur
            self.cur += 1
            return self.slots[i % self.n], (self.prefix, i % self.n)

    xin = [alloc([128, D], F32) for _ in range(2)]
    for b in range(NF // 128):
        xi = xin[b % 2]
        DMA("sp", xi, x_in[b * 128:(b + 1) * 128, :], "dx" if b % 2 == 0 else "dx2", [], [("xin", b % 2)])
        for half in range(2):
            bk = 4 * (b % 2) + 2 * half
            for c in range(4):
                cc = half * 4 + c
                TR(bank(bk)[:, c * 128:(c + 1) * 128], xi[:, cc * 128:(cc + 1) * 128], ident32,
                   [("xin", b % 2), "ident32"], BK(bk))
            CP("act" if half == 0 else "dve", xT[:, half * 4:half * 4 + 4, b * 128:(b + 1) * 128],
               bank(bk).rearrange("p (c t) -> p c t", c=4), BK(bk), xk(range(half * 4, half * 4 + 4), b * 128, 128))
    epoch()

    tiles0 = [(0, 128)] + [(128 + 512 * i, 512) for i in range(4)]

    tiles1 = [(128, 512), (0, 128)] + [(128 + 512 * i, 512) for i in range(1, 4)]
    if stop_after >= 1:
        W1 = 512
        hTs = [alloc([128, 8, W1], BF16) for _ in range(2)]
        sq = alloc([128, 8, W1], BF16)
        rstds = [alloc([128, W1], F32) for _ in range(2)]
        uT = alloc([128, 24, W1], BF16)
        vgs = [alloc([128, GH], BF16) for _ in range(4)]
        sjunk = alloc([128, 512], BF16)
        Ct = alloc([128, 24, 128], F32)
        tmpm_off = state["off"]
        tmpm = [alloc([128, 4, 128], F32) for _ in range(2)]
        wsTb = alloc([128, 8, 128], BF16)
        binv = alloc([1, GH], BF16)
        onesrow = alloc([1, 128], BF16)
        st1s = [alloc([128, 8], F32) for _ in range(4)]
        ssum = alloc([128, 4, 16], F32)
        w_in = W["gmlp_w_in"][0].rearrange("(k p) n -> p k n", p=128)
        w_out = W["gmlp_w_out"][0].rearrange("(m p) n -> p m n", p=128)
        scrA = nc.dram_tensor("scrA", [12, 128, 8 * 512], BF16).ap()
        scrC = nc.dram_tensor("scrC", [12, 128, 2 * 1024], BF16).ap()
        srcA, srcC = [], []
        for ti_, _ in enumerate(tiles1):
            for blk in (6, 7, 8, 9, 10, 11, 0, 1, 2, 3, 4, 5):
                col = blk * 512 if blk < 6 else GH + (blk - 6) * 512
                if ti_ == 0:
                    srcA.append(("w", w_in[:, :, col:col + 512], scrA[blk], ("scrA", blk)))
                else:
                    srcA.append(("r", scrA[blk], ("scrA", blk)))
            for mb in range(12):
                if ti_ == 0:
                    srcC.append(("w", w_out[:, mb * 2:(mb + 1) * 2, :], scrC[mb], ("scrC", mb)))
                else:
                    srcC.append(("r", scrC[mb], ("scrC", mb)))
        stA = Stream("wa", 3, [128, 8, 512], srcA)
        stC = Stream("wc", 2, [128, 2, 1024], srcC)
        off_save = state["off"]
        state["off"] = tmpm_off
        wsl = alloc([128, 128], F32)
        wsT32 = alloc([128, 128], F32)
        rsb = alloc([128, 128], F32)
        bsb = alloc([128, 128], F32)
        state["off"] = off_save
        MS("dve", onesrow, 1.0, ["onesrow"])
        DMA("pool", binv, W["gmlp_b_in"][0:1, GH:GF], "wb0", [], ["binv"])
        for g in range(8):
            DMA("sp", wsl, W["gmlp_w_s"][0, g], "dp", [], ["wsl"])
            P.add("pool", lambda h: h.affine_select(out=wsl, in_=wsl, compare_op=ALU.is_ge, fill=0.0, base=0,
                                                    pattern=[[-1, 128]], channel_multiplier=1),
                  reads=["wsl"], writes=["wsl"])
            TR(bank(0)[:, 0:128], wsl, ident32, ["wsl", "ident32"], BK(0))
            CP("act", wsT32, bank(0)[:, 0:128], BK(0), ["wsT32"])
            CP("dve", wsTb[:, g, :], wsT32, ["wsT32"], [("wsTb", g)])
            MM(bank(1)[:, 0:128], ones32, wsT32, True, True, ["ones32", "wsT32"], BK(1))
            CP("act", rsb, bank(1)[:, 0:128], BK(1), ["rsb"])
            DMA("sp", bsb, W["gmlp_b_s"][0, g].partition_broadcast(128), "dx", [], ["bsb"])
            for mm_ in range(3):
                m = g * 3 + mm_
                STT("dve", Ct[:, m, :], rsb, V["lnb"][:, m:m + 1], bsb, ALU.mult, ALU.add,
                    ["rsb", "bsb", "v_lnb"], [("C", m)])

        P.barrier(lambda h: h.memset(dummy_t, 0.0))
        npipe1 = NormPipe(tiles1, V["mix0"], hTs, sq, lambda i: rstds[i % 2][:, 0:tiles1[i][1]])
        npipe1.early(0)
        npipe1.late(0)
        for ti_, (t0, w) in enumerate(tiles1):
            nj = w // 128
            hT = hTs[ti_ % 2]
            hp = ti_ % 2
            npipe1.early(ti_ + 1)
            MS("dve", ssum, 0.0, ["ssum"])
            for blk in range(6):
                slot, key = stA.next()
                for j in range(nj):
                    bk = 4 + (blk * nj + j) % 4
                    for k in range(8):
                        MM(bank(bk), hT[:, k, j * 128:(j + 1) * 128], slot[:, k, :], k == 0, False,
                           [key, ("h", hp, k)], BK(bk))
                    MM(bank(bk), onesrow, binv[:, blk * 512:(blk + 1) * 512], False, True, ["onesrow", "binv"], BK(bk))
                    vblk = vgs[j][:, blk * 512:(blk + 1) * 512]
                    ACT(vblk, bank(bk), AF.Gelu_apprx_tanh, BK(bk) + ["ssum"], [("vg", j, blk)],
                        accum=ssum[:, j, blk:blk + 1])
                    ACT(sjunk, vblk, AF.Square, [("vg", j, blk), "ssum"], ["sjunk", ("vq", j, blk)],
                        accum=ssum[:, j, 8 + blk:9 + blk])
            for j in range(nj):
                vgk = [("vg", j, blk) for blk in range(6)]
                vqk = [("vq", j, blk) for blk in range(6)]
                st1 = st1s[j]
                sk = ("st1", j)
                P.add("dve", lambda h, j=j, st1=st1: h.tensor_reduce(out=st1[:, 0:1], in_=ssum[:, j, 0:6], axis=AX.X, op=ALU.add),
                      reads=vgk + ["ssum"], writes=[sk])
                P.add("dve", lambda h, j=j, st1=st1: h.tensor_reduce(out=st1[:, 6:7], in_=ssum[:, j, 8:14], axis=AX.X, op=ALU.add),
                      reads=vqk + ["ssum"], writes=[sk])
                TS("dve", st1[:, 0:1], st1[:, 0:1], 1.0 / GH, ALU.mult, [sk], [sk])
                TT("dve", st1[:, 1:2], st1[:, 0:1], st1[:, 0:1], ALU.mult, [sk], [sk])
                STT("dve", st1[:, 2:3], st1[:, 6:7], 1.0 / GH, st1[:, 1:2], ALU.mult, ALU.subtract, [sk], [sk])
                ACT(st1[:, 3:4], st1[:, 2:3], AF.Sqrt, [sk, "epst"], [sk], bias=epst)
                RECIP(st1[:, 4:5], st1[:, 3:4], [sk], [sk])
                STT("dve", st1[:, 5:6], st1[:, 0:1], -1.0, st1[:, 4:5], ALU.mult, ALU.mult, [sk], [sk])
                ACT(vgs[j], vgs[j], AF.Identity, vgk + [sk], [("vn", j)], bias=st1[:, 5:6], scale=st1[:, 4:5])
            for blk in range(6):
                if blk == 3:
                    npipe1.late(ti_ + 1)
                slot, key = stA.next()
                for mi in range(4):
                    m = blk * 4 + mi
                    bk = m % 4
                    for k in range(8):
                        MM(bank(bk)[:, 0:w], slot[:, k, mi * 128:(mi + 1) * 128], hT[:, k, 0:w], k == 0, k == 7,
                           [key, ("h", hp, k)], BK(bk))
                    ACT(uT[:, m, 0:w], bank(bk)[:, 0:w], AF.Gelu_apprx_tanh, BK(bk) + ["v_bin"], [("u", m)],
                        bias=V["bin"][:, m:m + 1])
            for j in range(nj):
                vnj = vgs[j]
                vnk = [("vn", j)] + [("vg", j, blk) for blk in range(6)]
                for mg in range(6):
                    bk = (j * 6 + mg) % 4
                    for mi in range(4):
                        m = mg * 4 + mi
                        g = m // 3
                        MM(bank(bk)[:, mi * 128:(mi + 1) * 128], vnj[:, m * 128:(m + 1) * 128], wsTb[:, g, :], True, True,
                           vnk + [("wsTb", g)], BK(bk))
                    tmi = (j * 6 + mg) % 2
                    tm = tmpm[tmi]
                    for mi in range(4):
                        m = mg * 4 + mi
                        STT("dve", tm[:, mi, :], bank(bk)[:, mi * 128:(mi + 1) * 128], V["lng"][:, m:m + 1], Ct[:, m, :],
                            ALU.mult, ALU.add, BK(bk) + [("C", m), "v_lng"], [("tm", tmi, mi)])
                    uk = [("u", mg * 4 + mi) for mi in range(4)]
                    usl = uT[:, mg * 4:mg * 4 + 4, j * 128:(j + 1) * 128]
                    TT("dve" if mg % 2 == 0 else "pool", usl, usl, tm, ALU.mult,
                       uk + [("tm", tmi, mi) for mi in range(4)], uk)
            for mb in range(12):
                slot, key = stC.next()
                for mi in range(2):
                    m = mb * 2 + mi
                    for n in range(8):
                        MM(bank(n)[:, 0:w], slot[:, mi, n * 128:(n + 1) * 128], uT[:, m, 0:w], m == 0, m == 23,
                           [key, ("u", m)], BK(n))
            for n in range(8):
                STT("dve", xT[:, n, t0:t0 + w], bank(n)[:, 0:w], V["bout"][:, n:n + 1], xT[:, n, t0:t0 + w],
                    ALU.add, ALU.add, BK(n) + xk([n], t0, w) + ["v_bout"], xk([n], t0, w))
        epoch()

    if stop_after >= 2:
        hTs = [alloc([128, 8, 512], BF16) for _ in range(2)]
        sq = alloc([128, 8, 512], BF16)
        rstds = [alloc([128, 512], F32) for _ in range(2)]
        actT = alloc([128, 22, 512], BF16)
        sg = [alloc([128, 512], F32) for _ in range(2)]
        wg = W["ffn_w_gate"][0].rearrange("(k p) n -> p k n", p=128)
        wu = W["ffn_w_up"][0].rearrange("(k p) n -> p k n", p=128)
        wd = W["ffn_w_down"][0].rearrange("(m p) n -> p m n", p=128)
        tiles2 = tiles1
        scrG = nc.dram_tensor("scrG", [11, 128, 8 * 256], BF16).ap()
        scrU = nc.dram_tensor("scrU", [11, 128, 8 * 256], BF16).ap()
        scrD = nc.dram_tensor("scrD", [11, 128, 2 * 1024], BF16).ap()
        srcA, srcB, srcC = [], [], []
        for ti_, _ in enumerate(tiles2):
            for blk in range(11):
                if ti_ == 0:
                    srcA.append(("w", wg[:, :, blk * 256:(blk + 1) * 256], scrG[blk], ("scrG", blk)))
                    srcB.append(("w", wu[:, :, blk * 256:(blk + 1) * 256], scrU[blk], ("scrU", blk)))
                else:
                    srcA.append(("r", scrG[blk], ("scrG", blk)))
                    srcB.append(("r", scrU[blk], ("scrU", blk)))
            for blk in range(11):
                if ti_ == 0:
                    srcC.append(("w", wd[:, blk * 2:(blk + 1) * 2, :], scrD[blk], ("scrD", blk)))
                else:
                    srcC.append(("r", scrD[blk], ("scrD", blk)))
        stA = Stream("wa", 3, [128, 8, 256], srcA)
        stB = Stream("wb", 3, [128, 8, 256], srcB)
        stC = Stream("wc", 3, [128, 2, 1024], srcC)
        npipe = NormPipe(tiles2, V["ffn0"], hTs, sq, lambda i: rstds[i % 2][:, 0:tiles2[i][1]])
        npipe.early(0)
        npipe.late(0)
        for ti_, (t0, w) in enumerate(tiles2):
            hT = hTs[ti_ % 2]
            hp = ti_ % 2
            npipe.early(ti_ + 1)
            for blk in range(11):
                if blk == 8:
                    npipe.late(ti_ + 1)
                sa, ka = stA.next()
                sb_, kb = stB.next()
                for fi in range(2):
                    f = blk * 2 + fi
                    bg = (f % 3) * 2
                    bu = bg + 1
                    for k in range(8):
                        MM(bank(bg)[:, 0:w], sa[:, k, fi * 128:(fi + 1) * 128], hT[:, k, 0:w], k == 0, k == 7,
                           [ka, ("h", hp, k)], BK(bg))
                    for k in range(8):
                        MM(bank(bu)[:, 0:w], sb_[:, k, fi * 128:(fi + 1) * 128], hT[:, k, 0:w], k == 0, k == 7,
                           [kb, ("h", hp, k)], BK(bu))
                    s = sg[f % 2]
                    ACT(s[:, 0:w], bank(bg)[:, 0:w], AF.Silu, BK(bg), [("sg", f % 2)])
                    TT("dve", actT[:, f, 0:w], s[:, 0:w], bank(bu)[:, 0:w], ALU.mult, [("sg", f % 2)] + BK(bu), [("act", f)])
            for mb in range(11):
                slot, key = stC.next()
                for mi in range(2):
                    f = mb * 2 + mi
                    for n in range(8):
                        MM(bank(n)[:, 0:w], slot[:, mi, n * 128:(n + 1) * 128], actT[:, f, 0:w], f == 0, f == 21,
                           [key, ("act", f)], BK(n))
            for n in range(8):
                TT("dve", xT[:, n, t0:t0 + w], bank(n)[:, 0:w], xT[:, n, t0:t0 + w], ALU.add,
                   BK(n) + xk([n], t0, w), xk([n], t0, w))
        epoch()

    def ple_stage(layer, p_src, tiles, p_off, gain):
        hTs = [alloc([128, 8, 512], BF16) for _ in range(2)]
        sq = alloc([128, 8, 512], BF16)
        rstds = [alloc([128, 512], F32) for _ in range(2)]
        wgt = alloc([128, 8, D], BF16)
        wpj = alloc([128, 2, D], BF16)
        pin = [alloc([128, PLE], F32) for _ in range(2)]
        pT = alloc([128, 2, 512], BF16)
        gs = [alloc([128, 512], F32) for _ in range(2)]
        tm = [alloc([128, 512], F32) for _ in range(2)]
        DMA("pool", wgt, W["ple_w_gate"][layer].rearrange("(k p) n -> p k n", p=128), "wa0", [], ["wgt"])
        DMA("pool", wpj, W["ple_w_proj"][layer].rearrange("(k p) n -> p k n", p=128), "wa1", [], ["wpj"])
        npipe = NormPipe(tiles, gain, hTs, sq, lambda i: rstds[i % 2][:, 0:tiles[i][1]])
        npipe.early(0)
        npipe.late(0)
        for ti_, (t0, w) in enumerate(tiles):
            hT = hTs[ti_ % 2]
            hp = ti_ % 2
            npipe.early(ti_ + 1)
            for j in range(w // 128):
                pi = pin[j % 2]
                r0 = t0 - p_off + j * 128
                DMA("sp", pi, p_src[r0:r0 + 128, :], "dp" if j % 2 == 0 else "dx", [], [("pin", j % 2)])
                for c in range(2):
                    TR(bank(6)[:, c * 128:(c + 1) * 128], pi[:, c * 128:(c + 1) * 128], ident32,
                       [("pin", j % 2), "ident32"], BK(6))
                CP("act", pT[:, :, j * 128:(j + 1) * 128], bank(6)[:, 0:256].rearrange("p (c t) -> p c t", c=2),
                   BK(6), [("pT", j)])
            PK = [("pT", j) for j in range(w // 128)]
            for n in range(8):
                if n == 4:
                    npipe.late(ti_ + 1)
                bg = (n % 3) * 2
                bp = bg + 1
                for k in range(8):
                    MM(bank(bg)[:, 0:w], wgt[:, k, n * 128:(n + 1) * 128], hT[:, k, 0:w], k == 0, k == 7,
                       ["wgt", ("h", hp, k)], BK(bg))
                for k in range(2):
                    MM(bank(bp)[:, 0:w], wpj[:, k, n * 128:(n + 1) * 128], pT[:, k, 0:w], k == 0, k == 1,
                       ["wpj"] + PK, BK(bp))
                g_ = gs[n % 2]
                t_ = tm[n % 2]
                ACT(g_[:, 0:w], bank(bg)[:, 0:w], AF.Sigmoid, BK(bg), [("gs", n % 2)])
                TT("dve", t_[:, 0:w], g_[:, 0:w], bank(bp)[:, 0:w], ALU.mult, [("gs", n % 2)] + BK(bp), [("tm", n % 2)])
                TT("pool", xT[:, n, t0:t0 + w], t_[:, 0:w], xT[:, n, t0:t0 + w], ALU.add,
                   [("tm", n % 2)] + xk([n], t0, w), xk([n], t0, w))
        epoch()

    if stop_after >= 3:
        ple_stage(0, p0_in, tiles0, 0, V["pleg0"])

    tiles_own = [(128 + 512 * i, 512) for i in range(4)]

    if stop_after >= 4:
        kT = alloc([64, 4, NF], BF16)
        Vt = alloc([128, NF // 128, 256], BF16)
        cosT = alloc([64, NF], F32)
        sinT = alloc([64, NF], F32)
        rstd_all = alloc([128, NF], F32)
        mark_save = state["mark"]
        state["mark"] = state["off"]
        posb = alloc([64, NF], F32)
        ang = alloc([64, NF], F32)
        kf = alloc([64, NF], F32)
        ki = alloc([64, NF], I32)
        fi32 = alloc([64, 1], I32)
        fq = alloc([64, 1], F32)
        sgn = alloc([64, 1], F32)
        DMA("sp", posb, pos_in.partition_broadcast(64), "dp", [], ["posb"])
        P.add("pool", lambda h: h.iota(fi32, pattern=[[0, 1]], base=0, channel_multiplier=1), writes=["fi32"])
        CP("dve", fq, fi32, ["fi32"], ["fq"])
        TS("dve", fq[32:64], fq[32:64], -32.0, ALU.add, ["fq"], ["fq"])
        ACT(fq, fq, AF.Exp, ["fq"], ["fq"], scale=-math.log(10000.0) / 32.0)
        MS("dve", sgn[0:32], -1.0, ["sgn"])
        MS("dve", sgn[32:64], 1.0, ["sgn"])
        TWO_PI = 2.0 * math.pi

        def sin_table(dst, shift, key):
            TS("dve", ang, posb, fq[:, 0:1], ALU.mult, ["posb", "fq"], ["ang"], s2=shift, op1=ALU.add)
            TS("dve", ki, ang, 1.0 / TWO_PI, ALU.mult, ["ang"], ["ki"])
            CP("dve", kf, ki, ["ki"], ["kf"])
            STT("dve", ang, kf, -TWO_PI, ang, ALU.mult, ALU.add, ["kf", "ang"], ["ang"])
            TS("dve", kf, ang, math.pi, ALU.is_gt, ["ang"], ["kf"], s2=-TWO_PI, op1=ALU.mult)
            TT("dve", ang, ang, kf, ALU.add, ["ang", "kf"], ["ang"])
            TS("dve", kf, ang, -math.pi, ALU.is_lt, ["ang"], ["kf"], s2=TWO_PI, op1=ALU.mult)
            TT("dve", ang, ang, kf, ALU.add, ["ang", "kf"], ["ang"])
            ACT(dst, ang, AF.Sin, ["ang"], [key])

        sin_table(cosT, math.pi / 2.0, "cosT")
        sin_table(sinT, 0.0, "sinT")
        TS("dve", sinT, sinT, sgn[:, 0:1], ALU.mult, ["sinT", "sgn"], ["sinT"])
        epoch_keep = True
        hTs = [alloc([128, 8, 512], BF16) for _ in range(2)]
        sq = alloc([128, 8, 512], BF16)
        wk = alloc([128, 8, 256], BF16)
        wks = alloc([128, 8, 256], BF16)
        wv = alloc([128, 8, 256], BF16)
        bk_t = alloc([64, 4], F32)
        bks_t = alloc([64, 4], F32)
        bvb = alloc([128, 256], F32)
        ra = [alloc([64, 512], F32) for _ in range(2)]
        rb = [alloc([64, 512], F32) for _ in range(2)]
        wkv_v = W["w_kv"].rearrange("(k p) n -> p k n", p=128)
        DMA("pool", wk, wkv_v[:, :, 0:256], "wa0", [], ["wk"])
        DMA("pool", wv, wkv_v[:, :, 256:512], "wa1", [], ["wv"])
        wk_v = wk.rearrange("p k (g two j) -> p k g two j", two=2, j=32)
        wks_v = wks.rearrange("p k (g two j) -> p k g two j", two=2, j=32)
        for k in range(8):
            CP("pool", wks_v[:, k, :, 0, :], wk_v[:, k, :, 1, :], ["wk"], [("wks", k, 0)])
            CP("pool", wks_v[:, k, :, 1, :], wk_v[:, k, :, 0, :], ["wk"], [("wks", k, 1)])
        bkv = W["b_kv"]
        load_vecT(bk_t, bkv[0:256].rearrange("(g p) -> g p", p=64), 4, 64, "bk_t")
        bkh = bkv[0:256].rearrange("(g two j) -> g two j", two=2, j=32)
        DMA("sp", vstage[0:4, 0:32], bkh[:, 1, :], "dc", [], ["vstage"])
        DMA("sp", vstage[0:4, 32:64], bkh[:, 0, :], "dc", [], ["vstage"])
        TR(bank(0)[0:64, 0:4], vstage[0:4, 0:64], ident32[0:4, 0:4], ["vstage", "ident32"], BK(0))
        CP("dve", bks_t, bank(0)[0:64, 0:4], BK(0), ["bks_t"])
        DMA("sp", bvb, bkv[256:512].partition_broadcast(128), "dc", [], ["bvb"])
        npipe = NormPipe(tiles0, V["kvg"], hTs, sq, lambda i: rstd_all[:, tiles0[i][0]:tiles0[i][0] + tiles0[i][1]])
        npipe.early(0)
        npipe.late(0)
        for ti_, (t0, w) in enumerate(tiles0):
            hT = hTs[ti_ % 2]
            hp = ti_ % 2
            npipe.early(ti_ + 1)
            for g in range(4):
                if g == 2:
                    npipe.late(ti_ + 1)
                b0 = (g % 2) * 2
                for k in range(8):
                    MM(bank(b0)[0:64, 0:w], wk[:, k, g * 64:(g + 1) * 64], hT[:, k, 0:w], k == 0, k == 7,
                       ["wk", ("h", hp, k)], BK(b0))
                for k in range(8):
                    MM(bank(b0 + 1)[0:64, 0:w], wks[:, k, g * 64:(g + 1) * 64], hT[:, k, 0:w], k == 0, k == 7,
                       [("wks", k, 0), ("wks", k, 1), ("h", hp, k)], BK(b0 + 1))
                a_ = ra[g % 2]
                b_ = rb[g % 2]
                STT("dve", a_[:, 0:w], bank(b0)[0:64, 0:w], bk_t[:, g:g + 1], cosT[:, t0:t0 + w], ALU.add, ALU.mult,
                    BK(b0) + ["bk_t", "cosT"], [("ra", g % 2)])
                STT("dve", b_[:, 0:w], bank(b0 + 1)[0:64, 0:w], bks_t[:, g:g + 1], sinT[:, t0:t0 + w], ALU.add, ALU.mult,
                    BK(b0 + 1) + ["bks_t", "sinT"], [("rb", g % 2)])
                TT("pool", kT[:, g, t0:t0 + w], a_[:, 0:w], b_[:, 0:w], ALU.add, [("ra", g % 2), ("rb", g % 2)],
                   [("kT", g, b) for b in range(t0 // 128, (t0 + w) // 128)])
            for j in range(w // 128):
                blk = t0 // 128 + j
                bkk = 4 + j % 2
                for k in range(8):
                    MM(bank(bkk)[:, 0:256], hT[:, k, j * 128:(j + 1) * 128], wv[:, k, :], k == 0, k == 7,
                       ["wv", ("h", hp, k)], BK(bkk))
                TT("dve", Vt[:, blk, :], bank(bkk)[:, 0:256], bvb, ALU.add, BK(bkk) + ["bvb"], [("V", blk)])
        epoch()
        WQ = 256
        hT = alloc([128, 8, WQ], BF16)
        wq = alloc([128, 8, D], BF16)
        wqs = alloc([128, 8, D], BF16)
        wo = alloc([128, 8, D], BF16)
        bq_t = alloc([64, 16], F32)
        bqs_t = alloc([64, 16], F32)
        sinkb = alloc([128, 16], F32)
        qT = alloc([64, 16, WQ], BF16)
        attnT = alloc([128, 8, WQ], BF16)
        ra = [alloc([64, WQ], F32)] * 2
        rb = [alloc([64, WQ], F32)] * 2
        maskt = alloc([128, 256], F32)
        mask0 = alloc([128, 256], F32)
        hmt = alloc([128, 128], F32)
        sms = [alloc([128, 4, 256], F32) for _ in range(2)]
        Pb = alloc([128, 4, 256], BF16)
        PTs = alloc([128, 8, 128], BF16)
        mxs = [alloc([128, 4], F32) for _ in range(2)]
        nmxs = [alloc([128, 4], F32) for _ in range(2)]
        rs_all = [alloc([128, 16], F32) for _ in range(2)]
        es_all = [alloc([128, 16], F32) for _ in range(2)]
        rinv = alloc([128, 16], F32)
        ao = alloc([128, 16, 64], BF16)
        wq_v = W["attn_w_q"][0].rearrange("(k p) n -> p k n", p=128)
        DMA("pool", wq, wq_v, "wa0", [], ["wq"])
        DMA("pool", wo, W["attn_w_o"][0].rearrange("(k p) n -> p k n", p=128), "wa1", [], ["wo"])
        wqv = wq.rearrange("p k (g two j) -> p k g two j", two=2, j=32)
        wqs_v = wqs.rearrange("p k (g two j) -> p k g two j", two=2, j=32)
        for k in range(8):
            CP("pool", wqs_v[:, k, :, 0, :], wqv[:, k, :, 1, :], ["wq"], [("wqs", k, 0)])
            CP("pool", wqs_v[:, k, :, 1, :], wqv[:, k, :, 0, :], ["wq"], [("wqs", k, 1)])
        bq = W["attn_b_q"][0]
        load_vecT(bq_t, bq.rearrange("(g p) -> g p", p=64), 16, 64, "bq_t")
        bqh = bq.rearrange("(g two j) -> g two j", two=2, j=32)
        DMA("sp", vstage[0:16, 0:32], bqh[:, 1, :], "dc", [], ["vstage"])
        DMA("sp", vstage[0:16, 32:64], bqh[:, 0, :], "dc", [], ["vstage"])
        TR(bank(0)[0:64, 0:16], vstage[0:16, 0:64], ident32[0:16, 0:16], ["vstage", "ident32"], BK(0))
        CP("dve", bqs_t, bank(0)[0:64, 0:16], BK(0), ["bqs_t"])
        DMA("sp", sinkb, W["attn_sinks"][0].partition_broadcast(128), "dc", [], ["sinkb"])
        DMA("sp", hmt, hm_in, "dc", [], ["hmt"])
        MS("dve", maskt, 0.0, ["maskt"])
        P.add("pool", lambda h: h.affine_select(out=maskt, in_=maskt, compare_op=ALU.is_ge, fill=NEG, base=-1,
                                                pattern=[[1, 256]], channel_multiplier=-1),
              reads=["maskt"], writes=["maskt"])
        P.add("pool", lambda h: h.affine_select(out=maskt, in_=maskt, compare_op=ALU.is_ge, fill=NEG, base=128,
                                                pattern=[[-1, 256]], channel_multiplier=1),
              reads=["maskt"], writes=["maskt"])
        CP("dve", mask0, maskt, ["maskt"], ["mask0"])
        TT("dve", mask0[:, 0:128], mask0[:, 0:128], hmt, ALU.add, ["mask0", "hmt"], ["mask0"])

        for ti in range(OWN // WQ):
            t0 = 128 + ti * WQ
            w = WQ
            rms_apply(t0, w, rstd_all[:, t0:t0 + w], V["mix1"], hT, "h")
            for hd in range(16):
                b0 = (hd % 2) * 2
                for k in range(8):
                    MM(bank(b0)[0:64, 0:w], wq[:, k, hd * 64:(hd + 1) * 64], hT[:, k, 0:w], k == 0, k == 7,
                       ["wq", ("h", k)], BK(b0))
                for k in range(8):
                    MM(bank(b0 + 1)[0:64, 0:w], wqs[:, k, hd * 64:(hd + 1) * 64], hT[:, k, 0:w], k == 0, k == 7,
                       [("wqs", k, 0), ("wqs", k, 1), ("h", k)], BK(b0 + 1))
                a_ = ra[0]
                b_ = rb[0]
                STT("dve", a_, bank(b0)[0:64, 0:w], bq_t[:, hd:hd + 1], cosT[:, t0:t0 + w], ALU.add, ALU.mult,
                    BK(b0) + ["bq_t", "cosT"], [("ra", 0)])
                STT("dve", b_, bank(b0 + 1)[0:64, 0:w], bqs_t[:, hd:hd + 1], sinT[:, t0:t0 + w], ALU.add, ALU.mult,
                    BK(b0 + 1) + ["bqs_t", "sinT"], [("rb", 0)])
                TT("pool", qT[:, hd, :], a_, b_, ALU.add, [("ra", 0), ("rb", 0)], [("q", hd)])
            iters = [(jb, g) for jb in range(w // 128) for g in range(4)]

            def phaseA(i):
                jb, g = iters[i]
                fb = t0 // 128 + jb
                mk = mask0 if fb == 1 else maskt
                mkey = "mask0" if fb == 1 else "maskt"
                par = i % 2
                bp = jb % 2
                sb0 = par * 2
                Sv = PS[:, 512 * sb0:512 * sb0 + 1024].rearrange("p (a b) -> p a b", a=4)
                for hh in range(4):
                    hd = 4 * g + hh
                    MM(PS[:, 512 * sb0 + hh * 256:512 * sb0 + (hh + 1) * 256], qT[:, hd, jb * 128:(jb + 1) * 128],
                       kT[:, g, (fb - 1) * 128:(fb + 1) * 128], True, True,
                       [("q", hd), ("kT", g, fb - 1), ("kT", g, fb)], BK(sb0, 2))
                STT("dve", sms[par], Sv, 0.125, mk.unsqueeze(1).to_broadcast([128, 4, 256]), ALU.mult, ALU.add,
                    BK(sb0, 2) + [mkey], [("sm", par)])
                P.add("dve", lambda h: h.tensor_reduce(out=mxs[par], in_=sms[par], axis=AX.X, op=ALU.max),
                      reads=[("sm", par)], writes=[("mx", par)])
                TT("dve", mxs[par], mxs[par], sinkb[:, 4 * g:4 * g + 4], ALU.max, [("mx", par), "sinkb"], [("mx", par)])
                TS("dve", nmxs[par], mxs[par], -1.0, ALU.mult, [("mx", par)], [("nmx", par)])
                if g == 0:
                    MS("dve", rs_all[bp], 0.0, [("rs", bp)] + [("rsc", bp, c) for c in range(16)])
                TT("dve", es_all[bp][:, 4 * g:4 * g + 4], sinkb[:, 4 * g:4 * g + 4], mxs[par], ALU.subtract,
                   ["sinkb", ("mx", par)], [("es", bp, g)])

            def phaseB(i):
                jb, g = iters[i]
                par = i % 2
                bp = jb % 2
                for hh in range(4):
                    ACT(Pb[:, hh, :], sms[par][:, hh, :], AF.Exp, [("sm", par), ("nmx", par), ("rs", bp)],
                        [("P", hh), ("rsc", bp, 4 * g + hh)],
                        bias=nmxs[par][:, hh:hh + 1], accum=rs_all[bp][:, 4 * g + hh:4 * g + hh + 1])
                ACT(es_all[bp][:, 4 * g:4 * g + 4], es_all[bp][:, 4 * g:4 * g + 4], AF.Exp, [("es", bp, g)], [("es", bp, g)])

            def phaseC(i):
                jb, g = iters[i]
                fb = t0 // 128 + jb
                bp = jb % 2
                ptb = bankb(4)
                for hh in range(4):
                    for half in range(2):
                        TR(ptb[:, (hh * 2 + half) * 128:(hh * 2 + half + 1) * 128], Pb[:, hh, half * 128:(half + 1) * 128],
                           identb, [("P", hh), "identb"], BK(4))
                CP("act" if i % 2 == 0 else "dve", PTs, ptb.rearrange("p (a b) -> p a b", a=8), BK(4), ["PTs"])
                for hh in range(4):
                    hd = 4 * g + hh
                    for half in range(2):
                        MM(PS[:, 512 * 5 + hd * 64:512 * 5 + (hd + 1) * 64], PTs[:, hh * 2 + half, :],
                           Vt[:, fb - 1 + half, g * 64:(g + 1) * 64], half == 0, half == 1,
                           ["PTs", ("V", fb - 1 + half)], BK(5, 2))
                if g == 3:
                    TT("dve", es_all[bp], es_all[bp], rs_all[bp], ALU.add,
                       [("es", bp, gg) for gg in range(4)] + [("rsc", bp, c) for c in range(16)], [("es", bp, gg) for gg in range(4)])
                    RECIP(rinv, es_all[bp], [("es", bp, gg) for gg in range(4)], ["rinv"])
                    Ov = PS[:, 512 * 5:512 * 7].rearrange("p (a b) -> p a b", a=16)
                    TT("dve", ao, Ov, rinv.unsqueeze(2).to_broadcast([128, 16, 64]), ALU.mult,
                       BK(5, 2) + ["rinv"], ["ao"])
                    aof = ao.rearrange("p a b -> p (a b)")
                    atb = bankb(7)
                    for c in range(8):
                        TR(atb[:, c * 128:(c + 1) * 128], aof[:, c * 128:(c + 1) * 128], identb, ["ao", "identb"], BK(7))
                    CP("act", attnT[:, :, jb * 128:(jb + 1) * 128], atb.rearrange("p (a b) -> p a b", a=8), BK(7),
                       [("attnT", jb)])

            phaseA(0)
            for i in range(len(iters)):
                if i + 1 < len(iters):
                    phaseA(i + 1)
                phaseB(i)
                phaseC(i)
            AK = [("attnT", jb) for jb in range(w // 128)]
            for n in range(8):
                bk = n % 4
                for k in range(8):
                    MM(bank(bk)[:, 0:w], wo[:, k, n * 128:(n + 1) * 128], attnT[:, k, :], k == 0, k == 7,
                       ["wo"] + AK, BK(bk))
                STT("dve", xT[:, n, t0:t0 + w], bank(bk)[:, 0:w], V["bo"][:, n:n + 1], xT[:, n, t0:t0 + w],
                    ALU.add, ALU.add, BK(bk) + xk([n], t0, w) + ["v_bo"], xk([n], t0, w))
        state["mark"] = mark_save
        epoch()

    if stop_after >= 5:
        hn = alloc([128, 8, OWN], BF16)
        cmb = alloc([128, 16, NE], F32)
        srcA, srcB, srcC = [], [], []
        for e in range(NE):
            wg = W["moe_w_gate"][0, e].rearrange("(k p) n -> p k n", p=128)
            wu = W["moe_w_up"][0, e].rearrange("(k p) n -> p k n", p=128)
            wd = W["moe_w_down"][0, e].rearrange("(m p) n -> p m n", p=128)
            for fg in range(7):
                srcA.append(wg[:, :, fg * 512:(fg + 1) * 512])
                srcB.append(wu[:, :, fg * 512:(fg + 1) * 512])
                srcC.append(wd[:, fg * 4:(fg + 1) * 4, :])
        stA = Stream("wa", 3, [128, 8, 512], srcA)
        stB = Stream("wb", 3, [128, 8, 512], srcB)
        stC = Stream("wc", 3, [128, 4, 1024], srcC)
        stA.prefetch()
        stB.prefetch()
        stC.prefetch()
        mark_save5 = state["mark"]
        state["mark"] = state["off"]
        sq = alloc([128, 8, 512], BF16)
        rstd = alloc([128, 512], F32)
        hn32 = alloc([128, 8, 128], F32)
        wr = alloc([128, 8, NE], F32)
        lg = alloc([128, NE], F32)
        top8 = alloc([128, 8], F32)
        mk8 = alloc([128, NE], F32)
        ex8 = alloc([128, NE], F32)
        den = alloc([128, 2], F32)
        DMA("sp", wr, W["moe_w_router"][0].rearrange("(k p) e -> p k e", p=128), "dc", [], ["wr"])
        for (t0, w) in tiles_own:
            rms_stats(t0, w, sq, rstd, 7)
            o0 = t0 - 128
            for c in range(8):
                STT("dve", hn[:, c, o0:o0 + w], xT[:, c, t0:t0 + w], V["ffn1"][:, c:c + 1],
                    rstd[:, 0:w], ALU.mult, ALU.mult, xk([c], t0, w) + ["rstd"], [("hn", c, o0 // 512)])
            for j in range(4):
                jj = o0 // 128 + j
                for c in range(8):
                    STT("dve", hn32[:, c, :], xT[:, c, t0 + j * 128:t0 + (j + 1) * 128],
                        V["ffn1"][:, c:c + 1], rstd[:, j * 128:(j + 1) * 128], ALU.mult, ALU.mult,
                        xk([c], t0 + j * 128, 128) + ["rstd"], [("hn32", c)])
                for c in range(8):
                    MM(bank(6)[:, 0:NE], hn32[:, c, :], wr[:, c, :], c == 0, c == 7, [("hn32", c), "wr"], BK(6))
                CP("act", lg, bank(6)[:, 0:NE], BK(6), ["lg"])
                P.add("dve", lambda h: h.max(out=top8, in_=lg), reads=["lg"], writes=["top8"])
                TS("dve", mk8, lg, top8[:, 1:2], ALU.is_ge, ["lg", "top8"], ["mk8"])
                TS("dve", den[:, 1:2], top8[:, 0:1], -1.0, ALU.mult, ["top8"], ["den"])
                ACT(ex8, lg, AF.Exp, ["lg", "den"], ["ex8"], bias=den[:, 1:2])
                TT("dve", ex8, ex8, mk8, ALU.mult, ["ex8", "mk8"], ["ex8"])
                P.add("dve", lambda h: h.tensor_reduce(out=den[:, 0:1], in_=ex8, axis=AX.X, op=ALU.add),
                      reads=["ex8"], writes=["den"])
                RECIP(den[:, 0:1], den[:, 0:1], ["den"], ["den"])
                TS("dve", cmb[:, jj, :], ex8, den[:, 0:1], ALU.mult, ["ex8", "den"], [("cmb", jj)])
        epoch()
        cbs = [alloc([128, OWN], F32) for _ in range(2)]
        dg = [alloc([128, 128], F32) for _ in range(2)]
        sg = [alloc([128, 256], F32) for _ in range(2)]
        tg = [alloc([128, 256], F32) for _ in range(2)]
        actE = [alloc([128, 4, 256], BF16) for _ in range(2)]

        def emit_cb(e):
            cb = cbs[e % 2]
            for jj in range(16):
                d_ = dg[jj % 2]
                TS("dve", d_, ident32, cmb[:, jj, e:e + 1], ALU.mult, ["ident32", ("cmb", jj)], [("dg", jj % 2)])
                MM(bank(7)[:, (jj % 4) * 128:(jj % 4 + 1) * 128], ones32, d_, True, True, ["ones32", ("dg", jj % 2)], BK(7))
                if jj % 4 == 3:
                    CP("act", cb[:, (jj - 3) * 128:(jj + 1) * 128], bank(7), BK(7), [("cb", e % 2, jj // 4)])

        def emit_gu(e, sa, ka, sb_, kb, ti):
            o0 = ti * 256
            ae = actE[ti % 2]
            cb = cbs[e % 2]
            for fb in range(4):
                bk = (ti * 4 + fb) % 4
                pg = bank(bk)[:, 0:256]
                pu = bank(bk)[:, 256:512]
                for k in range(8):
                    MM(pg, sa[:, k, fb * 128:(fb + 1) * 128], hn[:, k, o0:o0 + 256], k == 0, k == 7,
                       [ka, ("hn", k, o0 // 512)], BK(bk))
                for k in range(8):
                    MM(pu, sb_[:, k, fb * 128:(fb + 1) * 128], hn[:, k, o0:o0 + 256], k == 0, k == 7,
                       [kb, ("hn", k, o0 // 512)], BK(bk))
                s_ = sg[fb % 2]
                t_ = tg[fb % 2]
                ACT(s_, pg, AF.Silu, BK(bk), [("sg", fb % 2)])
                TT("dve", t_, s_, pu, ALU.mult, [("sg", fb % 2)] + BK(bk), [("tg", fb % 2)])
                TT("pool", ae[:, fb, :], t_, cb[:, o0:o0 + 256], ALU.mult, [("tg", fb % 2), ("cb", e % 2, o0 // 512)],
                   [("ae", ti % 2, fb)])

        def emit_down(sc, kc, ti):
            o0 = ti * 256
            ae = actE[ti % 2]
            for n in range(8):
                yb = 4 + n // 2
                yv = bank(yb)[:, (n % 2) * 256:(n % 2 + 1) * 256]
                for fb in range(4):
                    MM(yv, sc[:, fb, n * 128:(n + 1) * 128], ae[:, fb, :], fb == 0, fb == 3,
                       [kc, ("ae", ti % 2, fb)], BK(yb))
            for n2 in range(4):
                yv = bank(4 + n2).rearrange("p (a b) -> p a b", a=2)
                xv = xT[:, 2 * n2:2 * n2 + 2, 128 + o0:128 + o0 + 256]
                TT("dve", xv, yv, xv, ALU.add, BK(4 + n2) + xk([2 * n2, 2 * n2 + 1], 128 + o0, 256),
                   xk([2 * n2, 2 * n2 + 1], 128 + o0, 256))

        emit_cb(0)
        for e in range(NE):
            for fg in range(7):
                sa, ka = stA.next()
                sb_, kb = stB.next()
                sc, kc = stC.next()
                for ti in range(8):
                    emit_gu(e, sa, ka, sb_, kb, ti)
                    if ti > 0:
                        emit_down(sc, kc, ti - 1)
                    if fg == 3 and ti == 3 and e + 1 < NE:
                        emit_cb(e + 1)
                emit_down(sc, kc, 7)
        state["mark"] = mark_save5
        epoch()

    if stop_after >= 6:
        ple_stage(1, p1_in, tiles_own, 128, V["pleg1"])

    gb = alloc([128, D], F32)
    DMA("sp", gb, W["final_norm_g"].partition_broadcast(128), "dc", [], ["gb"])
    yo = [alloc([128, D], F32) for _ in range(2)]
    junk = alloc([128, D], F32)
    ss = alloc([128, 4], F32)
    outs = []
    for b in range(OWN // 128):
        t0 = 128 + b * 128
        pb = 4 * (b % 2)
        for half in range(2):
            for c in range(4):
                cc = half * 4 + c
                TR(PS[:, (pb + 2 * half) * 512 + c * 128:(pb + 2 * half) * 512 + (c + 1) * 128], xT[:, cc, t0:t0 + 128], ident32,
                   xk([cc], t0, 128) + ["ident32"], BK(pb + 2 * half))
        src = [PS[:, (pb + 2 * half) * 512:(pb + 2 * half) * 512 + 512] for half in range(2)]
        y_ = yo[b % 2]
        if final_norm:
            MS("dve", ss, 0.0, ["ss"])
            for half in range(2):
                ACT(junk[:, half * 512:(half + 1) * 512], src[half], AF.Square, BK(pb + 2 * half) + ["ss"], ["junk", "ss"],
                    accum=ss[:, half:half + 1])
            TT("dve", ss[:, 2:3], ss[:, 0:1], ss[:, 1:2], ALU.add, ["ss"], ["ss"])
            ACT(ss[:, 2:3], ss[:, 2:3], AF.Sqrt, ["ss", "epst"], ["ss"], bias=epst, scale=1.0 / D)
            RECIP(ss[:, 3:4], ss[:, 2:3], ["ss"], ["ss"])
            for half in range(2):
                STT("dve", y_[:, half * 512:(half + 1) * 512], src[half], ss[:, 3:4], gb[:, half * 512:(half + 1) * 512],
                    ALU.mult, ALU.mult, BK(pb + 2 * half) + ["ss", "gb"], [("yo", b % 2)])
        else:
            for half in range(2):
                CP("dve", y_[:, half * 512:(half + 1) * 512], src[half], BK(pb + 2 * half), [("yo", b % 2)])
        outs.append(DMA("sp", y_out[b * 128:(b + 1) * 128, :], y_, "dout" if b % 2 == 0 else "dout2", [("yo", b % 2)], []))

    P.emit_all(nc, sems, final_waits=outs[-2:])
    return nc, es


_CACHE = {}


def _core_inputs(x, p, c):
    b = c // 2
    half = c % 2
    s0 = half * OWN
    xc = np.zeros((NF, D), np.float32)
    p0c = np.zeros((NF, PLE), np.float32)
    if half == 1:
        xc[:] = x[b, s0 - HALO:s0 + OWN]
        p0c[:] = p[0, b, s0 - HALO:s0 + OWN]
    else:
        xc[HALO:] = x[b, 0:OWN]
        p0c[HALO:] = p[0, b, 0:OWN]
    p1c = np.ascontiguousarray(p[1, b, s0:s0 + OWN])
    pos = np.maximum(np.arange(s0 - HALO, s0 + OWN), 0).astype(np.float32)
    hm = np.full((128, 128), 0.0 if half == 1 else NEG, np.float32)
    return {"xc": xc, "p0c": p0c, "p1c": p1c, "pos": pos, "hm": hm}


def kernel(**inputs):
    x = np.asarray(inputs["x"], np.float32)
    p = np.asarray(inputs["p"], np.float32)
    if "nc" not in _CACHE:
        _CACHE["nc"] = build()
    nc, _es = _CACHE["nc"]
    wmap = {name: np.ascontiguousarray(np.asarray(inputs[name], np.float32)) for name, _ in WEIGHT_SPECS}
    in_maps = []
    for c in range(NCORES):
        m = dict(wmap)
        m.update(_core_inputs(x, p, c))
        in_maps.append(m)
    res = run_bass_kernel_spmd(nc, in_maps, core_ids=list(range(NCORES)))
    out = np.zeros((4, SEQ, D), np.float32)
    for c in range(NCORES):
        b, half = c // 2, c % 2
        out[b, half * OWN:(half + 1) * OWN] = res.results[c]["y"]
    return out
```

```python
import math
import os
from contextlib import ExitStack

import numpy as np
import concourse.bass as bass
import concourse.mybir as mybir
from concourse.bass_utils import run_bass_kernel_spmd

F32 = mybir.dt.float32
BF16 = mybir.dt.bfloat16
I32 = mybir.dt.int32
AF = mybir.ActivationFunctionType
ALU = mybir.AluOpType
AX = mybir.AxisListType

NCORES = 8
D = 1024
SEQ = 4096
OWN = 2048
HALO = 128
NF = OWN + HALO
GF = 6144
GH = 3072
DFF = 2816
NE = 8
DFE = 3584
PLE = 256
EPS = 1e-6
NEG = -1e30

ENGS = ("pe", "act", "dve", "pool", "sp")


class Op:
    __slots__ = ("eng", "emit", "deps", "sem", "seq", "signal", "is_dma")

    def __init__(self, eng, emit, is_dma):
        self.eng = eng
        self.emit = emit
        self.deps = []
        self.sem = None
        self.seq = 0
        self.signal = False
        self.is_dma = is_dma


class Prog:
    def __init__(self):
        self.ops = {e: [] for e in ENGS}
        self.last_write = {}
        self.readers = {}
        self.dma_tot = {}
        self.epoch_op = None
        self.shared = {"dc%d" % i for i in range(8)}

    def add(self, eng, emit, reads=(), writes=(), dma_sem=None):
        op = Op(eng, emit, dma_sem is not None)
        deps = {}
        for k in reads:
            w = self.last_write.get(k)
            if w is not None:
                deps[id(w)] = w
        for k in writes:
            w = self.last_write.get(k)
            if w is not None:
                deps[id(w)] = w
            for r in self.readers.get(k, ()):
                deps[id(r)] = r
        if self.epoch_op is not None:
            deps[id(self.epoch_op)] = self.epoch_op
        for k in reads:
            self.readers.setdefault(k, []).append(op)
        for k in writes:
            self.last_write[k] = op
            self.readers[k] = []
        for d in deps.values():
            if d is op:
                continue
            if (not d.is_dma) and d.eng == eng and eng in ("pe", "sp"):
                continue
            if d.is_dma and d.sem in self.shared:
                op.deps.append((d, self.dma_tot[d.sem]))
            else:
                op.deps.append((d, None))
            d.signal = True
        if dma_sem is not None:
            op.sem = dma_sem
            self.dma_tot[dma_sem] = self.dma_tot.get(dma_sem, 0) + 16
            op.seq = self.dma_tot[dma_sem]
        self.ops[eng].append(op)
        return op

    def barrier(self, emit):
        op = Op("dve", emit, False)
        seen = {}
        for e in ENGS:
            lst = self.ops[e]
            last_nd = None
            for o in lst:
                if o.is_dma:
                    seen[id(o)] = o
                else:
                    last_nd = o
            if last_nd is not None:
                seen[id(last_nd)] = last_nd
        newest = {}
        for o in seen.values():
            if o.is_dma:
                if o.sem not in newest or newest[o.sem].seq < o.seq:
                    newest[o.sem] = o
        for o in seen.values():
            if o.is_dma and newest[o.sem] is not o:
                continue
            op.deps.append((o, None))
            o.signal = True
        self.ops["dve"].append(op)
        self.epoch_op = op
        self.last_write = {}
        self.readers = {}
        return op

    def emit_all(self, nc, sems, final_waits=()):
        for e in ENGS:
            c = 0
            for op in self.ops[e]:
                if op.is_dma:
                    continue
                if op.signal:
                    c += 1
                    op.seq = c
                    op.sem = e

        def run(e, h):
            waited = {}
            for op in self.ops[e]:
                for d, ov in op.deps:
                    v = d.seq if ov is None else ov
                    if waited.get(d.sem, 0) < v:
                        h.wait_ge(sems[d.sem], v)
                        waited[d.sem] = v
                inst = op.emit(h)
                if op.is_dma:
                    inst.then_inc(sems[op.sem], 16)
                elif op.signal:
                    inst.then_inc(sems[op.sem], 1)
            if e == "sp":
                for d in final_waits:
                    if waited.get(d.sem, 0) < d.seq:
                        h.wait_ge(sems[d.sem], d.seq)
                        waited[d.sem] = d.seq

        with nc.Block() as block:
            @block.tensor
            def _(h):
                run("pe", h)

            @block.scalar
            def _(h):
                run("act", h)

            @block.vector
            def _(h):
                run("dve", h)

            @block.gpsimd
            def _(h):
                run("pool", h)

            @block.sync
            def _(h):
                run("sp", h)


WEIGHT_SPECS = [
    ("mix_norm_g", [2, D]), ("ffn_norm_g", [2, D]),
    ("gmlp_w_in", [1, D, GF]), ("gmlp_b_in", [1, GF]), ("gmlp_ln_g", [1, GH]), ("gmlp_ln_b", [1, GH]),
    ("gmlp_w_s", [1, 8, 128, 128]), ("gmlp_b_s", [1, 8, 128]), ("gmlp_w_out", [1, GH, D]),
    ("gmlp_b_out", [1, D]), ("kv_norm_g", [D]), ("w_kv", [D, 512]), ("b_kv", [512]),
    ("attn_w_q", [1, D, D]), ("attn_b_q", [1, D]), ("attn_sinks", [1, 16]), ("attn_w_o", [1, D, D]),
    ("attn_b_o", [1, D]), ("ffn_w_gate", [1, D, DFF]), ("ffn_w_up", [1, D, DFF]),
    ("ffn_w_down", [1, DFF, D]), ("moe_w_router", [1, D, NE]), ("moe_w_gate", [1, NE, D, DFE]),
    ("moe_w_up", [1, NE, D, DFE]), ("moe_w_down", [1, NE, DFE, D]), ("ple_w_proj", [2, PLE, D]),
    ("ple_norm_g", [2, D]), ("ple_w_gate", [2, D, D]), ("final_norm_g", [D]),
]


def build(stop_after=99, final_norm=True):
    nc = bass.Bass("TRN2", target_bir_lowering=False)
    W = {}
    for name, shp in WEIGHT_SPECS:
        W[name] = nc.dram_tensor(name, shp, F32, kind="ExternalInput").ap()
    x_in = nc.dram_tensor("xc", [NF, D], F32, kind="ExternalInput").ap()
    p0_in = nc.dram_tensor("p0c", [NF, PLE], F32, kind="ExternalInput").ap()
    p1_in = nc.dram_tensor("p1c", [OWN, PLE], F32, kind="ExternalInput").ap()
    pos_in = nc.dram_tensor("pos", [NF], F32, kind="ExternalInput").ap()
    hm_in = nc.dram_tensor("hm", [128, 128], F32, kind="ExternalInput").ap()
    y_out = nc.dram_tensor("y", [OWN, D], F32, kind="ExternalOutput").ap()

    P = Prog()
    es = ExitStack()
    ARENA_B = 206 * 1024
    arena = es.enter_context(nc.sbuf_tensor("arena", [128, ARENA_B // 2], BF16))
    PS = es.enter_context(nc.psum_tensor("ps", [128, 4096], F32))
    sem_names = list(ENGS) + ["dx", "dx2", "dp", "dc", "dout", "dout2"] + ["dc%d" % i for i in range(8)] + ["wa%d" % i for i in range(4)] + \
        ["wb%d" % i for i in range(4)] + ["wc%d" % i for i in range(4)] + ["wr0", "wr1", "wr2", "wr3", "xwa0", "xwa1", "xwa2", "xwc0", "xwc1", "hwa0", "hwa1", "hwa2", "hwc0", "hwc1", "xwb0", "xwb1", "xwb2", "hwb0", "hwb1", "hwb2", "xwc2", "hwc2"]
    sems = {n: es.enter_context(nc.semaphore(n)) for n in sem_names}

    state = {"off": 0, "mark": 0}

    def alloc(shape, dt, parts=128):
        nb = int(np.prod(shape[1:])) * (4 if dt in (F32, I32) else 2)
        nb = (nb + 63) // 64 * 64
        off = state["off"]
        assert off + nb <= ARENA_B, ("arena overflow", off, nb)
        state["off"] = off + nb
        v = arena[:, off // 2:(off + nb) // 2]
        if dt in (F32, I32):
            v = v.bitcast(dt)
        n = int(np.prod(shape[1:]))
        v = v[:, 0:n]
        if len(shape) == 3:
            v = v.rearrange("p (a b) -> p a b", a=shape[1])
        elif len(shape) == 4:
            v = v.rearrange("p (a b c) -> p a b c", a=shape[1], b=shape[2])
        if shape[0] < 128:
            v = v[0:shape[0]]
        return v

    def bank(i, n=1):
        return PS[:, 512 * i:512 * (i + n)]

    def bankb(i):
        return PS[:, 512 * i:512 * (i + 1)].bitcast(BF16)

    def BK(i, n=1):
        return ["B%d" % j for j in range(i, i + n)]

    def MM(out, lhsT, rhs, start, stop, r, w):
        P.add("pe", lambda h: h.matmul(out, lhsT, rhs, start=start, stop=stop), reads=r, writes=w)

    def TR(out, in_, ident, r, w):
        P.add("pe", lambda h: h.transpose(out=out, in_=in_, identity=ident), reads=r, writes=w)

    def ACT(out, in_, func, r, w, bias=None, scale=1.0, accum=None):
        kw = {}
        if bias is not None:
            kw["bias"] = bias
        if accum is not None:
            kw["accum_out"] = accum
        P.add("act", lambda h: h.activation(out=out, in_=in_, func=func, scale=scale, **kw), reads=r, writes=w)

    def TT(eng, out, in0, in1, op, r, w):
        P.add(eng, lambda h: h.tensor_tensor(out=out, in0=in0, in1=in1, op=op), reads=r, writes=w)

    def TS(eng, out, in0, s1, op0, r, w, s2=None, op1=None):
        if op1 is None:
            P.add(eng, lambda h: h.tensor_scalar(out=out, in0=in0, scalar1=s1, scalar2=None, op0=op0), reads=r, writes=w)
        else:
            P.add(eng, lambda h: h.tensor_scalar(out=out, in0=in0, scalar1=s1, scalar2=s2, op0=op0, op1=op1), reads=r, writes=w)

    def STT(eng, out, in0, scalar, in1, op0, op1, r, w):
        P.add(eng, lambda h: h.scalar_tensor_tensor(out=out, in0=in0, scalar=scalar, in1=in1, op0=op0, op1=op1),
              reads=r, writes=w)

    def CP(eng, out, in_, r, w):
        if eng == "act":
            P.add("act", lambda h: h.copy(out=out, in_=in_), reads=r, writes=w)
        else:
            P.add(eng, lambda h: h.tensor_copy(out=out, in_=in_), reads=r, writes=w)

    def MS(eng, ap, val, w):
        P.add(eng, lambda h: h.memset(ap, val), writes=w)

    dc_rot = [0]

    def DMA(eng, out, in_, sem, r, w, slow=False):
        if sem == "dc":
            sem = "dc%d" % (dc_rot[0] % 8)
            dc_rot[0] += 1
        if slow:
            return P.add(eng, lambda h: h.dma_start(out=out, in_=in_, allow_slow_non_contiguous=True),
                         reads=r, writes=w, dma_sem=sem)
        return P.add(eng, lambda h: h.dma_start(out=out, in_=in_), reads=r, writes=w, dma_sem=sem)

    def RECIP(out, in_, r, w):
        P.add("dve", lambda h: h.reciprocal(out=out, in_=in_), reads=r, writes=w)

    def epoch():
        dummy = alloc_dummy[0]
        P.barrier(lambda h: h.memset(dummy, 0.0))
        state["off"] = state["mark"]

    xT = alloc([128, 8, NF], F32)
    ident32 = alloc([128, 128], F32)
    ones32 = alloc([128, 128], F32)
    identb = alloc([128, 128], BF16)
    onesb = alloc([128, 128], BF16)
    epst = alloc([128, 1], F32)
    pit = alloc([128, 1], F32)
    dummy_t = alloc([128, 16], F32)
    alloc_dummy = [dummy_t]
    vec_specs = [("mix0", W["mix_norm_g"][0], 8), ("mix1", W["mix_norm_g"][1], 8),
                 ("ffn0", W["ffn_norm_g"][0], 8), ("ffn1", W["ffn_norm_g"][1], 8),
                 ("pleg0", W["ple_norm_g"][0], 8), ("pleg1", W["ple_norm_g"][1], 8),
                 ("kvg", W["kv_norm_g"], 8), ("fing", W["final_norm_g"], 8),
                 ("bin", W["gmlp_b_in"][0], 48), ("lng", W["gmlp_ln_g"][0], 24), ("lnb", W["gmlp_ln_b"][0], 24),
                 ("bout", W["gmlp_b_out"][0], 8), ("bo", W["attn_b_o"][0], 8)]
    V = {}
    for nm, src, nch in vec_specs:
        V[nm] = alloc([128, nch], F32)
    vstage = alloc([48, 128], F32)

    MS("dve", ident32, 0.0, ["ident32"])
    P.add("pool", lambda h: h.affine_select(out=ident32, in_=ident32, compare_op=ALU.not_equal, fill=1.0,
                                            base=0, pattern=[[-1, 128]], channel_multiplier=1),
          reads=["ident32"], writes=["ident32"])
    CP("dve", identb, ident32, ["ident32"], ["identb"])
    MS("dve", ones32, 1.0, ["ones32"])
    MS("dve", onesb, 1.0, ["onesb"])
    MS("dve", epst, EPS, ["epst"])
    MS("dve", pit, math.pi, ["pit"])
    def load_vecT(dst, src2d, n, pp, key):
        DMA("sp", vstage[0:n, 0:pp], src2d, "dc", [], ["vstage"])
        TR(bank(0)[0:pp, 0:n], vstage[0:n, 0:pp], ident32[0:n, 0:n], ["vstage", "ident32"], BK(0))
        CP("dve", dst, bank(0)[0:pp, 0:n], BK(0), [key])

    for nm, src, nch in vec_specs:
        load_vecT(V[nm], src.rearrange("(c p) -> c p", p=128), nch, 128, "v_" + nm)
    state["mark"] = state["off"]

    def xk(ns, t0, w):
        return [("x", n, b) for n in ns for b in range(t0 // 128, (t0 + w) // 128)]

    ALLN = list(range(8))

    def rms_stats(t0, w, sq, rstd, bk):
        for c in range(8):
            TT("dve" if c % 2 == 0 else "pool", sq[:, c, 0:w], xT[:, c, t0:t0 + w], xT[:, c, t0:t0 + w], ALU.mult,
               xk([c], t0, w), [("sq", c)])
        for c in range(8):
            MM(bank(bk)[:, 0:w], onesb, sq[:, c, 0:w], c == 0, c == 7, ["onesb", ("sq", c)], BK(bk))
        ACT(rstd[:, 0:w], bank(bk)[:, 0:w], AF.Sqrt, BK(bk) + ["epst"], ["rstd"], bias=epst, scale=1.0 / D)
        RECIP(rstd[:, 0:w], rstd[:, 0:w], ["rstd"], ["rstd"])

    def rms_apply(t0, w, rstd, gain, dst, dkey, engs=("dve",)):
        for c in range(8):
            STT(engs[c % len(engs)], dst[:, c, 0:w], xT[:, c, t0:t0 + w], gain[:, c:c + 1], rstd[:, 0:w],
                ALU.mult, ALU.mult, xk([c], t0, w) + ["rstd"], [(dkey, c)])

    class NormPipe:
        def __init__(self, tiles, gain, hTs, sq, rstd_of, bk=7):
            self.tiles, self.gain, self.hTs, self.sq, self.rstd_of, self.bk = tiles, gain, hTs, sq, rstd_of, bk

        def early(self, i):
            if i >= len(self.tiles):
                return
            t0, w = self.tiles[i]
            for c in range(8):
                TT("dve" if c % 2 == 0 else "pool", self.sq[:, c, 0:w], xT[:, c, t0:t0 + w], xT[:, c, t0:t0 + w],
                   ALU.mult, xk([c], t0, w), [("sq", c)])

        def late(self, i):
            if i >= len(self.tiles):
                return
            t0, w = self.tiles[i]
            bk = self.bk
            rstd = self.rstd_of(i)
            rk = ("rstd", i % 2)
            for c in range(8):
                MM(bank(bk)[:, 0:w], onesb, self.sq[:, c, 0:w], c == 0, c == 7, ["onesb", ("sq", c)], BK(bk))
            ACT(rstd, bank(bk)[:, 0:w], AF.Sqrt, BK(bk) + ["epst"], [rk], bias=epst, scale=1.0 / D)
            RECIP(rstd, rstd, [rk], [rk])
            dst = self.hTs[i % 2]
            for c in range(8):
                STT("dve", dst[:, c, 0:w], xT[:, c, t0:t0 + w], self.gain[:, c:c + 1], rstd,
                    ALU.mult, ALU.mult, xk([c], t0, w) + [rk], [("h", i % 2, c)])

    class Stream:
        def __init__(self, prefix, nslots, shape, srcs):
            self.slots = [alloc(shape, BF16) for _ in range(nslots)]
            self.prefix = prefix
            self.n = nslots
            self.srcs = srcs
            self.issued = 0
            self.cur = 0

        def _issue(self):
            i = self.issued
            slot = self.slots[i % self.n]
            src = self.srcs[i]
            sem = "%s%d" % (self.prefix, i % self.n)
            key = (self.prefix, i % self.n)
            if isinstance(src, tuple) and src[0] == "r":
                DMA("sp", slot.rearrange("p a b -> p (a b)"), src[1], "h" + sem, [src[2]], [key])
            elif isinstance(src, tuple) and src[0] == "w":
                DMA("pool", slot, src[1], sem, [], [key])
                DMA("sp", src[2], slot.rearrange("p a b -> p (a b)"), "x" + sem, [key], [src[3]])
            else:
                dst = slot
                if len(src.shape) == 3 and tuple(src.shape) != tuple(slot.shape):
                    dst = slot[:, 0:src.shape[1], 0:src.shape[2]]
                DMA("pool", dst, src, sem, [], [key])
            self.issued += 1

        def prefetch(self):
            while self.issued < len(self.srcs) and self.issued < self.cur + self.n:
                self._issue()

        def next(self):
            while self.issued < len(self.srcs) and self.issued < self.cur + self.n:
                self._issue()
            i = self.cur
            self.cur += 1
            return self.slots[i % self.n], (self.prefix, i % self.n)

    xin = [alloc([128, D], F32) for _ in range(4)]
    for b in range(NF // 128):
        xi = xin[b % 4]
        DMA("sp", xi, x_in[b * 128:(b + 1) * 128, :], ("dx", "dx2", "dp", "wr0")[b % 4], [], [("xin", b % 4)])
        for half in range(2):
            bk = 4 * (b % 2) + 2 * half
            for c in range(4):
                cc = half * 4 + c
                TR(bank(bk)[:, c * 128:(c + 1) * 128], xi[:, cc * 128:(cc + 1) * 128], ident32,
                   [("xin", b % 4), "ident32"], BK(bk))
            CP("act" if half == 0 else "dve", xT[:, half * 4:half * 4 + 4, b * 128:(b + 1) * 128],
               bank(bk).rearrange("p (c t) -> p c t", c=4), BK(bk), xk(range(half * 4, half * 4 + 4), b * 128, 128))
    epoch()

    tiles0 = [(0, 128)] + [(128 + 512 * i, 512) for i in range(4)]

    tiles1 = [(128, 512), (0, 128)] + [(128 + 512 * i, 512) for i in range(1, 4)]
    if stop_after >= 1:
        W1 = 512
        hTs = [alloc([128, 8, W1], BF16) for _ in range(2)]
        sq = alloc([128, 8, W1], BF16)
        rstds = [alloc([128, W1], F32) for _ in range(2)]
        uT = alloc([128, 24, W1], BF16)
        vgs = [alloc([128, GH], BF16) for _ in range(4)]
        sjunk = alloc([128, 512], BF16)
        Ct = alloc([128, 24, 128], F32)
        tmpm_off = state["off"]
        tmpm = [alloc([128, 4, 128], F32) for _ in range(2)]
        wsTb = alloc([128, 8, 128], BF16)
        binv = alloc([1, GH], BF16)
        onesrow = alloc([1, 128], BF16)
        st1s = [alloc([128, 8], F32) for _ in range(4)]
        ssum = alloc([128, 4, 16], F32)
        w_in = W["gmlp_w_in"][0].rearrange("(k p) n -> p k n", p=128)
        w_out = W["gmlp_w_out"][0].rearrange("(m p) n -> p m n", p=128)
        scrA = nc.dram_tensor("scrA", [12, 128, 8 * 512], BF16).ap()
        scrC = nc.dram_tensor("scrC", [12, 128, 2 * 1024], BF16).ap()
        srcA, srcC = [], []
        for ti_, _ in enumerate(tiles1):
            for blk in (6, 7, 8, 9, 10, 11, 0, 1, 2, 3, 4, 5):
                col = blk * 512 if blk < 6 else GH + (blk - 6) * 512
                if ti_ == 0:
                    srcA.append(("w", w_in[:, :, col:col + 512], scrA[blk], ("scrA", blk)))
                else:
                    srcA.append(("r", scrA[blk], ("scrA", blk)))
            for mb in range(12):
                if ti_ == 0:
                    srcC.append(("w", w_out[:, mb * 2:(mb + 1) * 2, :], scrC[mb], ("scrC", mb)))
                else:
                    srcC.append(("r", scrC[mb], ("scrC", mb)))
        stA = Stream("wa", 3, [128, 8, 512], srcA)
        stC = Stream("wc", 2, [128, 2, 1024], srcC)
        off_save = state["off"]
        state["off"] = tmpm_off
        wsl = alloc([128, 128], F32)
        wsT32 = alloc([128, 128], F32)
        rsb = alloc([128, 128], F32)
        bsb = alloc([128, 128], F32)
        state["off"] = off_save
        MS("dve", onesrow, 1.0, ["onesrow"])
        DMA("pool", binv, W["gmlp_b_in"][0:1, GH:GF], "wb0", [], ["binv"])
        for g in range(8):
            DMA("sp", wsl, W["gmlp_w_s"][0, g], "dp", [], ["wsl"])
            P.add("pool", lambda h: h.affine_select(out=wsl, in_=wsl, compare_op=ALU.is_ge, fill=0.0, base=0,
                                                    pattern=[[-1, 128]], channel_multiplier=1),
                  reads=["wsl"], writes=["wsl"])
            TR(bank(0)[:, 0:128], wsl, ident32, ["wsl", "ident32"], BK(0))
            CP("act", wsT32, bank(0)[:, 0:128], BK(0), ["wsT32"])
            CP("dve", wsTb[:, g, :], wsT32, ["wsT32"], [("wsTb", g)])
            MM(bank(1)[:, 0:128], ones32, wsT32, True, True, ["ones32", "wsT32"], BK(1))
            CP("act", rsb, bank(1)[:, 0:128], BK(1), ["rsb"])
            DMA("sp", bsb, W["gmlp_b_s"][0, g].partition_broadcast(128), "dx", [], ["bsb"])
            for mm_ in range(3):
                m = g * 3 + mm_
                STT("dve", Ct[:, m, :], rsb, V["lnb"][:, m:m + 1], bsb, ALU.mult, ALU.add,
                    ["rsb", "bsb", "v_lnb"], [("C", m)])

        P.barrier(lambda h: h.memset(dummy_t, 0.0))
        npipe1 = NormPipe(tiles1, V["mix0"], hTs, sq, lambda i: rstds[i % 2][:, 0:tiles1[i][1]])
        npipe1.early(0)
        npipe1.late(0)
        for ti_, (t0, w) in enumerate(tiles1):
            nj = w // 128
            hT = hTs[ti_ % 2]
            hp = ti_ % 2
            npipe1.early(ti_ + 1)
            MS("dve", ssum, 0.0, ["ssum"])
            for blk in range(6):
                slot, key = stA.next()
                for j in range(nj):
                    bk = 4 + (blk * nj + j) % 4
                    for k in range(8):
                        MM(bank(bk), hT[:, k, j * 128:(j + 1) * 128], slot[:, k, :], k == 0, False,
                           [key, ("h", hp, k)], BK(bk))
                    MM(bank(bk), onesrow, binv[:, blk * 512:(blk + 1) * 512], False, True, ["onesrow", "binv"], BK(bk))
                    vblk = vgs[j][:, blk * 512:(blk + 1) * 512]
                    ACT(vblk, bank(bk), AF.Gelu_apprx_tanh, BK(bk) + ["ssum"], [("vg", j, blk)],
                        accum=ssum[:, j, blk:blk + 1])
                    ACT(sjunk, vblk, AF.Square, [("vg", j, blk), "ssum"], ["sjunk", ("vq", j, blk)],
                        accum=ssum[:, j, 8 + blk:9 + blk])
            for j in range(nj):
                vgk = [("vg", j, blk) for blk in range(6)]
                vqk = [("vq", j, blk) for blk in range(6)]
                st1 = st1s[j]
                sk = ("st1", j)
                P.add("dve", lambda h, j=j, st1=st1: h.tensor_reduce(out=st1[:, 0:1], in_=ssum[:, j, 0:6], axis=AX.X, op=ALU.add),
                      reads=vgk + ["ssum"], writes=[sk])
                P.add("dve", lambda h, j=j, st1=st1: h.tensor_reduce(out=st1[:, 6:7], in_=ssum[:, j, 8:14], axis=AX.X, op=ALU.add),
                      reads=vqk + ["ssum"], writes=[sk])
                TS("dve", st1[:, 0:1], st1[:, 0:1], 1.0 / GH, ALU.mult, [sk], [sk])
                TT("dve", st1[:, 1:2], st1[:, 0:1], st1[:, 0:1], ALU.mult, [sk], [sk])
                STT("dve", st1[:, 2:3], st1[:, 6:7], 1.0 / GH, st1[:, 1:2], ALU.mult, ALU.subtract, [sk], [sk])
                ACT(st1[:, 3:4], st1[:, 2:3], AF.Sqrt, [sk, "epst"], [sk], bias=epst)
                RECIP(st1[:, 4:5], st1[:, 3:4], [sk], [sk])
                STT("dve", st1[:, 5:6], st1[:, 0:1], -1.0, st1[:, 4:5], ALU.mult, ALU.mult, [sk], [sk])
                ACT(vgs[j], vgs[j], AF.Identity, vgk + [sk], [("vn", j)], bias=st1[:, 5:6], scale=st1[:, 4:5])
            for blk in range(6):
                if blk == 3:
                    npipe1.late(ti_ + 1)
                slot, key = stA.next()
                for mi in range(4):
                    m = blk * 4 + mi
                    bk = m % 4
                    for k in range(8):
                        MM(bank(bk)[:, 0:w], slot[:, k, mi * 128:(mi + 1) * 128], hT[:, k, 0:w], k == 0, k == 7,
                           [key, ("h", hp, k)], BK(bk))
                    ACT(uT[:, m, 0:w], bank(bk)[:, 0:w], AF.Gelu_apprx_tanh, BK(bk) + ["v_bin"], [("u", m)],
                        bias=V["bin"][:, m:m + 1])
            for j in range(nj):
                vnj = vgs[j]
                vnk = [("vn", j)] + [("vg", j, blk) for blk in range(6)]
                for mg in range(6):
                    bk = (j * 6 + mg) % 4
                    for mi in range(4):
                        m = mg * 4 + mi
                        g = m // 3
                        MM(bank(bk)[:, mi * 128:(mi + 1) * 128], vnj[:, m * 128:(m + 1) * 128], wsTb[:, g, :], True, True,
                           vnk + [("wsTb", g)], BK(bk))
                    tmi = (j * 6 + mg) % 2
                    tm = tmpm[tmi]
                    for mi in range(4):
                        m = mg * 4 + mi
                        STT("dve", tm[:, mi, :], bank(bk)[:, mi * 128:(mi + 1) * 128], V["lng"][:, m:m + 1], Ct[:, m, :],
                            ALU.mult, ALU.add, BK(bk) + [("C", m), "v_lng"], [("tm", tmi, mi)])
                    uk = [("u", mg * 4 + mi) for mi in range(4)]
                    usl = uT[:, mg * 4:mg * 4 + 4, j * 128:(j + 1) * 128]
                    TT("dve" if mg % 2 == 0 else "pool", usl, usl, tm, ALU.mult,
                       uk + [("tm", tmi, mi) for mi in range(4)], uk)
            for mb in range(12):
                slot, key = stC.next()
                for mi in range(2):
                    m = mb * 2 + mi
                    for n in range(8):
                        MM(bank(n)[:, 0:w], slot[:, mi, n * 128:(n + 1) * 128], uT[:, m, 0:w], m == 0, m == 23,
                           [key, ("u", m)], BK(n))
            for n in range(8):
                STT("dve", xT[:, n, t0:t0 + w], bank(n)[:, 0:w], V["bout"][:, n:n + 1], xT[:, n, t0:t0 + w],
                    ALU.add, ALU.add, BK(n) + xk([n], t0, w) + ["v_bout"], xk([n], t0, w))
        epoch()

    if stop_after >= 2:
        hTs = [alloc([128, 8, 512], BF16) for _ in range(2)]
        sq = alloc([128, 8, 512], BF16)
        rstds = [alloc([128, 512], F32) for _ in range(2)]
        actT = alloc([128, 22, 512], BF16)
        sg = [alloc([128, 512], F32) for _ in range(2)]
        wg = W["ffn_w_gate"][0].rearrange("(k p) n -> p k n", p=128)
        wu = W["ffn_w_up"][0].rearrange("(k p) n -> p k n", p=128)
        wd = W["ffn_w_down"][0].rearrange("(m p) n -> p m n", p=128)
        tiles2 = tiles1
        scrG = nc.dram_tensor("scrG", [11, 128, 8 * 256], BF16).ap()
        scrU = nc.dram_tensor("scrU", [11, 128, 8 * 256], BF16).ap()
        scrD = nc.dram_tensor("scrD", [11, 128, 2 * 1024], BF16).ap()
        srcA, srcB, srcC = [], [], []
        for ti_, _ in enumerate(tiles2):
            for blk in range(11):
                if ti_ == 0:
                    srcA.append(("w", wg[:, :, blk * 256:(blk + 1) * 256], scrG[blk], ("scrG", blk)))
                    srcB.append(("w", wu[:, :, blk * 256:(blk + 1) * 256], scrU[blk], ("scrU", blk)))
                else:
                    srcA.append(("r", scrG[blk], ("scrG", blk)))
                    srcB.append(("r", scrU[blk], ("scrU", blk)))
            for blk in range(11):
                if ti_ == 0:
                    srcC.append(("w", wd[:, blk * 2:(blk + 1) * 2, :], scrD[blk], ("scrD", blk)))
                else:
                    srcC.append(("r", scrD[blk], ("scrD", blk)))
        stA = Stream("wa", 3, [128, 8, 256], srcA)
        stB = Stream("wb", 3, [128, 8, 256], srcB)
        stC = Stream("wc", 3, [128, 2, 1024], srcC)
        npipe = NormPipe(tiles2, V["ffn0"], hTs, sq, lambda i: rstds[i % 2][:, 0:tiles2[i][1]])
        npipe.early(0)
        npipe.late(0)
        for ti_, (t0, w) in enumerate(tiles2):
            hT = hTs[ti_ % 2]
            hp = ti_ % 2
            npipe.early(ti_ + 1)
            for blk in range(11):
                if blk == 8:
                    npipe.late(ti_ + 1)
                sa, ka = stA.next()
                sb_, kb = stB.next()
                for fi in range(2):
                    f = blk * 2 + fi
                    bg = (f % 3) * 2
                    bu = bg + 1
                    for k in range(8):
                        MM(bank(bg)[:, 0:w], sa[:, k, fi * 128:(fi + 1) * 128], hT[:, k, 0:w], k == 0, k == 7,
                           [ka, ("h", hp, k)], BK(bg))
                    for k in range(8):
                        MM(bank(bu)[:, 0:w], sb_[:, k, fi * 128:(fi + 1) * 128], hT[:, k, 0:w], k == 0, k == 7,
                           [kb, ("h", hp, k)], BK(bu))
                    s = sg[f % 2]
                    ACT(s[:, 0:w], bank(bg)[:, 0:w], AF.Silu, BK(bg), [("sg", f % 2)])
                    TT("dve", actT[:, f, 0:w], s[:, 0:w], bank(bu)[:, 0:w], ALU.mult, [("sg", f % 2)] + BK(bu), [("act", f)])
            for mb in range(11):
                slot, key = stC.next()
                for mi in range(2):
                    f = mb * 2 + mi
                    for n in range(8):
                        MM(bank(n)[:, 0:w], slot[:, mi, n * 128:(n + 1) * 128], actT[:, f, 0:w], f == 0, f == 21,
                           [key, ("act", f)], BK(n))
            for n in range(8):
                TT("dve", xT[:, n, t0:t0 + w], bank(n)[:, 0:w], xT[:, n, t0:t0 + w], ALU.add,
                   BK(n) + xk([n], t0, w), xk([n], t0, w))
        epoch()

    def ple_stage(layer, p_src, tiles, p_off, gain):
        hTs = [alloc([128, 8, 512], BF16) for _ in range(2)]
        sq = alloc([128, 8, 512], BF16)
        rstds = [alloc([128, 512], F32) for _ in range(2)]
        wgt = alloc([128, 8, D], BF16)
        wpj = alloc([128, 2, D], BF16)
        pin = [alloc([128, PLE], F32) for _ in range(2)]
        pT = alloc([128, 2, 512], BF16)
        gs = [alloc([128, 512], F32) for _ in range(2)]
        tm = [alloc([128, 512], F32) for _ in range(2)]
        DMA("pool", wgt, W["ple_w_gate"][layer].rearrange("(k p) n -> p k n", p=128), "wa0", [], ["wgt"])
        DMA("pool", wpj, W["ple_w_proj"][layer].rearrange("(k p) n -> p k n", p=128), "wa1", [], ["wpj"])
        npipe = NormPipe(tiles, gain, hTs, sq, lambda i: rstds[i % 2][:, 0:tiles[i][1]])
        npipe.early(0)
        npipe.late(0)
        for ti_, (t0, w) in enumerate(tiles):
            hT = hTs[ti_ % 2]
            hp = ti_ % 2
            npipe.early(ti_ + 1)
            for j in range(w // 128):
                pi = pin[j % 2]
                r0 = t0 - p_off + j * 128
                DMA("sp", pi, p_src[r0:r0 + 128, :], "dp" if j % 2 == 0 else "dx", [], [("pin", j % 2)])
                for c in range(2):
                    TR(bank(6)[:, c * 128:(c + 1) * 128], pi[:, c * 128:(c + 1) * 128], ident32,
                       [("pin", j % 2), "ident32"], BK(6))
                CP("act", pT[:, :, j * 128:(j + 1) * 128], bank(6)[:, 0:256].rearrange("p (c t) -> p c t", c=2),
                   BK(6), [("pT", j)])
            PK = [("pT", j) for j in range(w // 128)]
            for n in range(8):
                if n == 4:
                    npipe.late(ti_ + 1)
                bg = (n % 3) * 2
                bp = bg + 1
                for k in range(8):
                    MM(bank(bg)[:, 0:w], wgt[:, k, n * 128:(n + 1) * 128], hT[:, k, 0:w], k == 0, k == 7,
                       ["wgt", ("h", hp, k)], BK(bg))
                for k in range(2):
                    MM(bank(bp)[:, 0:w], wpj[:, k, n * 128:(n + 1) * 128], pT[:, k, 0:w], k == 0, k == 1,
                       ["wpj"] + PK, BK(bp))
                g_ = gs[n % 2]
                t_ = tm[n % 2]
                ACT(g_[:, 0:w], bank(bg)[:, 0:w], AF.Sigmoid, BK(bg), [("gs", n % 2)])
                TT("dve", t_[:, 0:w], g_[:, 0:w], bank(bp)[:, 0:w], ALU.mult, [("gs", n % 2)] + BK(bp), [("tm", n % 2)])
                TT("pool", xT[:, n, t0:t0 + w], t_[:, 0:w], xT[:, n, t0:t0 + w], ALU.add,
                   [("tm", n % 2)] + xk([n], t0, w), xk([n], t0, w))
        epoch()

    if stop_after >= 3:
        ple_stage(0, p0_in, tiles0, 0, V["pleg0"])

    tiles_own = [(128 + 512 * i, 512) for i in range(4)]

    if stop_after >= 4:
        kT = alloc([64, 4, NF], BF16)
        Vt = alloc([128, NF // 128, 256], BF16)
        cosT = alloc([64, NF], F32)
        sinT = alloc([64, NF], F32)
        rstd_all = alloc([128, NF], F32)
        mark_save = state["mark"]
        state["mark"] = state["off"]
        posb = alloc([64, NF], F32)
        ang = alloc([64, NF], F32)
        kf = alloc([64, NF], F32)
        ki = alloc([64, NF], I32)
        fi32 = alloc([64, 1], I32)
        fq = alloc([64, 1], F32)
        sgn = alloc([64, 1], F32)
        DMA("sp", posb, pos_in.partition_broadcast(64), "dp", [], ["posb"])
        P.add("pool", lambda h: h.iota(fi32, pattern=[[0, 1]], base=0, channel_multiplier=1), writes=["fi32"])
        CP("dve", fq, fi32, ["fi32"], ["fq"])
        TS("dve", fq[32:64], fq[32:64], -32.0, ALU.add, ["fq"], ["fq"])
        ACT(fq, fq, AF.Exp, ["fq"], ["fq"], scale=-math.log(10000.0) / 32.0)
        MS("dve", sgn[0:32], -1.0, ["sgn"])
        MS("dve", sgn[32:64], 1.0, ["sgn"])
        TWO_PI = 2.0 * math.pi

        def sin_table(dst, shift, key):
            TS("dve", ang, posb, fq[:, 0:1], ALU.mult, ["posb", "fq"], ["ang"], s2=shift, op1=ALU.add)
            TS("dve", ki, ang, 1.0 / TWO_PI, ALU.mult, ["ang"], ["ki"])
            CP("dve", kf, ki, ["ki"], ["kf"])
            STT("dve", ang, kf, -TWO_PI, ang, ALU.mult, ALU.add, ["kf", "ang"], ["ang"])
            TS("dve", kf, ang, math.pi, ALU.is_gt, ["ang"], ["kf"], s2=-TWO_PI, op1=ALU.mult)
            TT("dve", ang, ang, kf, ALU.add, ["ang", "kf"], ["ang"])
            TS("dve", kf, ang, -math.pi, ALU.is_lt, ["ang"], ["kf"], s2=TWO_PI, op1=ALU.mult)
            TT("dve", ang, ang, kf, ALU.add, ["ang", "kf"], ["ang"])
            ACT(dst, ang, AF.Sin, ["ang"], [key])

        sin_table(cosT, math.pi / 2.0, "cosT")
        sin_table(sinT, 0.0, "sinT")
        TS("dve", sinT, sinT, sgn[:, 0:1], ALU.mult, ["sinT", "sgn"], ["sinT"])
        epoch_keep = True
        hTs = [alloc([128, 8, 512], BF16) for _ in range(2)]
        sq = alloc([128, 8, 512], BF16)
        wk = alloc([128, 8, 256], BF16)
        wks = alloc([128, 8, 256], BF16)
        wv = alloc([128, 8, 256], BF16)
        bk_t = alloc([64, 4], F32)
        bks_t = alloc([64, 4], F32)
        bvb = alloc([128, 256], F32)
        ra = [alloc([64, 512], F32) for _ in range(2)]
        rb = [alloc([64, 512], F32) for _ in range(2)]
        wkv_v = W["w_kv"].rearrange("(k p) n -> p k n", p=128)
        DMA("pool", wk, wkv_v[:, :, 0:256], "wa0", [], ["wk"])
        DMA("pool", wv, wkv_v[:, :, 256:512], "wa1", [], ["wv"])
        wk_v = wk.rearrange("p k (g two j) -> p k g two j", two=2, j=32)
        wks_v = wks.rearrange("p k (g two j) -> p k g two j", two=2, j=32)
        for k in range(8):
            CP("pool", wks_v[:, k, :, 0, :], wk_v[:, k, :, 1, :], ["wk"], [("wks", k, 0)])
            CP("pool", wks_v[:, k, :, 1, :], wk_v[:, k, :, 0, :], ["wk"], [("wks", k, 1)])
        bkv = W["b_kv"]
        load_vecT(bk_t, bkv[0:256].rearrange("(g p) -> g p", p=64), 4, 64, "bk_t")
        bkh = bkv[0:256].rearrange("(g two j) -> g two j", two=2, j=32)
        DMA("sp", vstage[0:4, 0:32], bkh[:, 1, :], "dc", [], ["vstage"])
        DMA("sp", vstage[0:4, 32:64], bkh[:, 0, :], "dc", [], ["vstage"])
        TR(bank(0)[0:64, 0:4], vstage[0:4, 0:64], ident32[0:4, 0:4], ["vstage", "ident32"], BK(0))
        CP("dve", bks_t, bank(0)[0:64, 0:4], BK(0), ["bks_t"])
        DMA("sp", bvb, bkv[256:512].partition_broadcast(128), "dc", [], ["bvb"])
        npipe = NormPipe(tiles0, V["kvg"], hTs, sq, lambda i: rstd_all[:, tiles0[i][0]:tiles0[i][0] + tiles0[i][1]])
        npipe.early(0)
        npipe.late(0)
        for ti_, (t0, w) in enumerate(tiles0):
            hT = hTs[ti_ % 2]
            hp = ti_ % 2
            npipe.early(ti_ + 1)
            for g in range(4):
                if g == 2:
                    npipe.late(ti_ + 1)
                b0 = (g % 2) * 2
                for k in range(8):
                    MM(bank(b0)[0:64, 0:w], wk[:, k, g * 64:(g + 1) * 64], hT[:, k, 0:w], k == 0, k == 7,
                       ["wk", ("h", hp, k)], BK(b0))
                for k in range(8):
                    MM(bank(b0 + 1)[0:64, 0:w], wks[:, k, g * 64:(g + 1) * 64], hT[:, k, 0:w], k == 0, k == 7,
                       [("wks", k, 0), ("wks", k, 1), ("h", hp, k)], BK(b0 + 1))
                a_ = ra[g % 2]
                b_ = rb[g % 2]
                STT("dve", a_[:, 0:w], bank(b0)[0:64, 0:w], bk_t[:, g:g + 1], cosT[:, t0:t0 + w], ALU.add, ALU.mult,
                    BK(b0) + ["bk_t", "cosT"], [("ra", g % 2)])
                STT("dve", b_[:, 0:w], bank(b0 + 1)[0:64, 0:w], bks_t[:, g:g + 1], sinT[:, t0:t0 + w], ALU.add, ALU.mult,
                    BK(b0 + 1) + ["bks_t", "sinT"], [("rb", g % 2)])
                TT("pool", kT[:, g, t0:t0 + w], a_[:, 0:w], b_[:, 0:w], ALU.add, [("ra", g % 2), ("rb", g % 2)],
                   [("kT", g, b) for b in range(t0 // 128, (t0 + w) // 128)])
            for j in range(w // 128):
                blk = t0 // 128 + j
                bkk = 4 + j % 2
                for k in range(8):
                    MM(bank(bkk)[:, 0:256], hT[:, k, j * 128:(j + 1) * 128], wv[:, k, :], k == 0, k == 7,
                       ["wv", ("h", hp, k)], BK(bkk))
                TT("dve", Vt[:, blk, :], bank(bkk)[:, 0:256], bvb, ALU.add, BK(bkk) + ["bvb"], [("V", blk)])
        epoch()
        WQ = 256
        hT = alloc([128, 8, WQ], BF16)
        wq = alloc([128, 8, D], BF16)
        wqs = alloc([128, 8, D], BF16)
        wo = alloc([128, 8, D], BF16)
        bq_t = alloc([64, 16], F32)
        bqs_t = alloc([64, 16], F32)
        sinkb = alloc([128, 16], F32)
        qT = alloc([64, 16, WQ], BF16)
        attnT = alloc([128, 8, WQ], BF16)
        ra = [alloc([64, WQ], F32)] * 2
        rb = [alloc([64, WQ], F32)] * 2
        maskt = alloc([128, 256], F32)
        mask0 = alloc([128, 256], F32)
        hmt = alloc([128, 128], F32)
        sms = [alloc([128, 4, 256], F32) for _ in range(2)]
        Pb = alloc([128, 4, 256], BF16)
        PTs = alloc([128, 8, 128], BF16)
        mxs = [alloc([128, 4], F32) for _ in range(2)]
        nmxs = [alloc([128, 4], F32) for _ in range(2)]
        rs_all = [alloc([128, 16], F32) for _ in range(2)]
        es_all = [alloc([128, 16], F32) for _ in range(2)]
        rinv = alloc([128, 16], F32)
        ao = alloc([128, 16, 64], BF16)
        wq_v = W["attn_w_q"][0].rearrange("(k p) n -> p k n", p=128)
        DMA("pool", wq, wq_v, "wa0", [], ["wq"])
        DMA("pool", wo, W["attn_w_o"][0].rearrange("(k p) n -> p k n", p=128), "wa1", [], ["wo"])
        wqv = wq.rearrange("p k (g two j) -> p k g two j", two=2, j=32)
        wqs_v = wqs.rearrange("p k (g two j) -> p k g two j", two=2, j=32)
        for k in range(8):
            CP("pool", wqs_v[:, k, :, 0, :], wqv[:, k, :, 1, :], ["wq"], [("wqs", k, 0)])
            CP("pool", wqs_v[:, k, :, 1, :], wqv[:, k, :, 0, :], ["wq"], [("wqs", k, 1)])
        bq = W["attn_b_q"][0]
        load_vecT(bq_t, bq.rearrange("(g p) -> g p", p=64), 16, 64, "bq_t")
        bqh = bq.rearrange("(g two j) -> g two j", two=2, j=32)
        DMA("sp", vstage[0:16, 0:32], bqh[:, 1, :], "dc", [], ["vstage"])
        DMA("sp", vstage[0:16, 32:64], bqh[:, 0, :], "dc", [], ["vstage"])
        TR(bank(0)[0:64, 0:16], vstage[0:16, 0:64], ident32[0:16, 0:16], ["vstage", "ident32"], BK(0))
        CP("dve", bqs_t, bank(0)[0:64, 0:16], BK(0), ["bqs_t"])
        DMA("sp", sinkb, W["attn_sinks"][0].partition_broadcast(128), "dc", [], ["sinkb"])
        DMA("sp", hmt, hm_in, "dc", [], ["hmt"])
        MS("dve", maskt, 0.0, ["maskt"])
        P.add("pool", lambda h: h.affine_select(out=maskt, in_=maskt, compare_op=ALU.is_ge, fill=NEG, base=-1,
                                                pattern=[[1, 256]], channel_multiplier=-1),
              reads=["maskt"], writes=["maskt"])
        P.add("pool", lambda h: h.affine_select(out=maskt, in_=maskt, compare_op=ALU.is_ge, fill=NEG, base=128,
                                                pattern=[[-1, 256]], channel_multiplier=1),
              reads=["maskt"], writes=["maskt"])
        CP("dve", mask0, maskt, ["maskt"], ["mask0"])
        TT("dve", mask0[:, 0:128], mask0[:, 0:128], hmt, ALU.add, ["mask0", "hmt"], ["mask0"])

        for ti in range(OWN // WQ):
            t0 = 128 + ti * WQ
            w = WQ
            rms_apply(t0, w, rstd_all[:, t0:t0 + w], V["mix1"], hT, "h")
            for hd in range(16):
                b0 = (hd % 2) * 2
                for k in range(8):
                    MM(bank(b0)[0:64, 0:w], wq[:, k, hd * 64:(hd + 1) * 64], hT[:, k, 0:w], k == 0, k == 7,
                       ["wq", ("h", k)], BK(b0))
                for k in range(8):
                    MM(bank(b0 + 1)[0:64, 0:w], wqs[:, k, hd * 64:(hd + 1) * 64], hT[:, k, 0:w], k == 0, k == 7,
                       [("wqs", k, 0), ("wqs", k, 1), ("h", k)], BK(b0 + 1))
                a_ = ra[0]
                b_ = rb[0]
                STT("dve", a_, bank(b0)[0:64, 0:w], bq_t[:, hd:hd + 1], cosT[:, t0:t0 + w], ALU.add, ALU.mult,
                    BK(b0) + ["bq_t", "cosT"], [("ra", 0)])
                STT("dve", b_, bank(b0 + 1)[0:64, 0:w], bqs_t[:, hd:hd + 1], sinT[:, t0:t0 + w], ALU.add, ALU.mult,
                    BK(b0 + 1) + ["bqs_t", "sinT"], [("rb", 0)])
                TT("pool", qT[:, hd, :], a_, b_, ALU.add, [("ra", 0), ("rb", 0)], [("q", hd)])
            iters = [(jb, g) for jb in range(w // 128) for g in range(4)]

            def phaseA(i):
                jb, g = iters[i]
                fb = t0 // 128 + jb
                mk = mask0 if fb == 1 else maskt
                mkey = "mask0" if fb == 1 else "maskt"
                par = i % 2
                bp = jb % 2
                sb0 = par * 2
                Sv = PS[:, 512 * sb0:512 * sb0 + 1024].rearrange("p (a b) -> p a b", a=4)
                for hh in range(4):
                    hd = 4 * g + hh
                    MM(PS[:, 512 * sb0 + hh * 256:512 * sb0 + (hh + 1) * 256], qT[:, hd, jb * 128:(jb + 1) * 128],
                       kT[:, g, (fb - 1) * 128:(fb + 1) * 128], True, True,
                       [("q", hd), ("kT", g, fb - 1), ("kT", g, fb)], BK(sb0, 2))
                STT("dve", sms[par], Sv, 0.125, mk.unsqueeze(1).to_broadcast([128, 4, 256]), ALU.mult, ALU.add,
                    BK(sb0, 2) + [mkey], [("sm", par)])
                P.add("dve", lambda h: h.tensor_reduce(out=mxs[par], in_=sms[par], axis=AX.X, op=ALU.max),
                      reads=[("sm", par)], writes=[("mx", par)])
                TT("dve", mxs[par], mxs[par], sinkb[:, 4 * g:4 * g + 4], ALU.max, [("mx", par), "sinkb"], [("mx", par)])
                TS("dve", nmxs[par], mxs[par], -1.0, ALU.mult, [("mx", par)], [("nmx", par)])
                if g == 0:
                    MS("dve", rs_all[bp], 0.0, [("rs", bp)] + [("rsc", bp, c) for c in range(16)])
                TT("dve", es_all[bp][:, 4 * g:4 * g + 4], sinkb[:, 4 * g:4 * g + 4], mxs[par], ALU.subtract,
                   ["sinkb", ("mx", par)], [("es", bp, g)])

            def phaseB(i):
                jb, g = iters[i]
                par = i % 2
                bp = jb % 2
                for hh in range(4):
                    ACT(Pb[:, hh, :], sms[par][:, hh, :], AF.Exp, [("sm", par), ("nmx", par), ("rs", bp)],
                        [("P", hh), ("rsc", bp, 4 * g + hh)],
                        bias=nmxs[par][:, hh:hh + 1], accum=rs_all[bp][:, 4 * g + hh:4 * g + hh + 1])
                ACT(es_all[bp][:, 4 * g:4 * g + 4], es_all[bp][:, 4 * g:4 * g + 4], AF.Exp, [("es", bp, g)], [("es", bp, g)])

            def phaseC(i):
                jb, g = iters[i]
                fb = t0 // 128 + jb
                bp = jb % 2
                ptb = bankb(4)
                for hh in range(4):
                    for half in range(2):
                        TR(ptb[:, (hh * 2 + half) * 128:(hh * 2 + half + 1) * 128], Pb[:, hh, half * 128:(half + 1) * 128],
                           identb, [("P", hh), "identb"], BK(4))
                CP("act" if i % 2 == 0 else "dve", PTs, ptb.rearrange("p (a b) -> p a b", a=8), BK(4), ["PTs"])
                for hh in range(4):
                    hd = 4 * g + hh
                    for half in range(2):
                        MM(PS[:, 512 * 5 + hd * 64:512 * 5 + (hd + 1) * 64], PTs[:, hh * 2 + half, :],
                           Vt[:, fb - 1 + half, g * 64:(g + 1) * 64], half == 0, half == 1,
                           ["PTs", ("V", fb - 1 + half)], BK(5, 2))
                if g == 3:
                    TT("dve", es_all[bp], es_all[bp], rs_all[bp], ALU.add,
                       [("es", bp, gg) for gg in range(4)] + [("rsc", bp, c) for c in range(16)], [("es", bp, gg) for gg in range(4)])
                    RECIP(rinv, es_all[bp], [("es", bp, gg) for gg in range(4)], ["rinv"])
                    Ov = PS[:, 512 * 5:512 * 7].rearrange("p (a b) -> p a b", a=16)
                    TT("dve", ao, Ov, rinv.unsqueeze(2).to_broadcast([128, 16, 64]), ALU.mult,
                       BK(5, 2) + ["rinv"], ["ao"])
                    aof = ao.rearrange("p a b -> p (a b)")
                    atb = bankb(7)
                    for c in range(8):
                        TR(atb[:, c * 128:(c + 1) * 128], aof[:, c * 128:(c + 1) * 128], identb, ["ao", "identb"], BK(7))
                    CP("act", attnT[:, :, jb * 128:(jb + 1) * 128], atb.rearrange("p (a b) -> p a b", a=8), BK(7),
                       [("attnT", jb)])

            phaseA(0)
            for i in range(len(iters)):
                if i + 1 < len(iters):
                    phaseA(i + 1)
                phaseB(i)
                phaseC(i)
            AK = [("attnT", jb) for jb in range(w // 128)]
            for n in range(8):
                bk = n % 4
                for k in range(8):
                    MM(bank(bk)[:, 0:w], wo[:, k, n * 128:(n + 1) * 128], attnT[:, k, :], k == 0, k == 7,
                       ["wo"] + AK, BK(bk))
                STT("dve", xT[:, n, t0:t0 + w], bank(bk)[:, 0:w], V["bo"][:, n:n + 1], xT[:, n, t0:t0 + w],
                    ALU.add, ALU.add, BK(bk) + xk([n], t0, w) + ["v_bo"], xk([n], t0, w))
        state["mark"] = mark_save
        epoch()

    if stop_after >= 5:
        hn = alloc([128, 8, OWN], BF16)
        cmb = alloc([128, 16, NE], F32)
        srcA, srcB, srcC = [], [], []
        for e in range(NE):
            wg = W["moe_w_gate"][0, e].rearrange("(k p) n -> p k n", p=128)
            wu = W["moe_w_up"][0, e].rearrange("(k p) n -> p k n", p=128)
            wd = W["moe_w_down"][0, e].rearrange("(m p) n -> p m n", p=128)
            for fg in range(7):
                srcA.append(wg[:, :, fg * 512:(fg + 1) * 512])
                srcB.append(wu[:, :, fg * 512:(fg + 1) * 512])
                srcC.append(wd[:, fg * 4:(fg + 1) * 4, :])
        stA = Stream("wa", 3, [128, 8, 512], srcA)
        stB = Stream("wb", 3, [128, 8, 512], srcB)
        stC = Stream("wc", 3, [128, 4, 1024], srcC)
        stA.prefetch()
        stB.prefetch()
        stC.prefetch()
        mark_save5 = state["mark"]
        state["mark"] = state["off"]
        sq = alloc([128, 8, 512], BF16)
        rstd = alloc([128, 512], F32)
        hn32 = alloc([128, 8, 128], F32)
        wr = alloc([128, 8, NE], F32)
        lg = alloc([128, NE], F32)
        top8 = alloc([128, 8], F32)
        mk8 = alloc([128, NE], F32)
        ex8 = alloc([128, NE], F32)
        den = alloc([128, 2], F32)
        DMA("sp", wr, W["moe_w_router"][0].rearrange("(k p) e -> p k e", p=128), "dc", [], ["wr"])
        for (t0, w) in tiles_own:
            rms_stats(t0, w, sq, rstd, 7)
            o0 = t0 - 128
            for c in range(8):
                STT("dve", hn[:, c, o0:o0 + w], xT[:, c, t0:t0 + w], V["ffn1"][:, c:c + 1],
                    rstd[:, 0:w], ALU.mult, ALU.mult, xk([c], t0, w) + ["rstd"], [("hn", c, o0 // 512)])
            for j in range(4):
                jj = o0 // 128 + j
                for c in range(8):
                    STT("dve", hn32[:, c, :], xT[:, c, t0 + j * 128:t0 + (j + 1) * 128],
                        V["ffn1"][:, c:c + 1], rstd[:, j * 128:(j + 1) * 128], ALU.mult, ALU.mult,
                        xk([c], t0 + j * 128, 128) + ["rstd"], [("hn32", c)])
                for c in range(8):
                    MM(bank(6)[:, 0:NE], hn32[:, c, :], wr[:, c, :], c == 0, c == 7, [("hn32", c), "wr"], BK(6))
                CP("act", lg, bank(6)[:, 0:NE], BK(6), ["lg"])
                P.add("dve", lambda h: h.max(out=top8, in_=lg), reads=["lg"], writes=["top8"])
                TS("dve", mk8, lg, top8[:, 1:2], ALU.is_ge, ["lg", "top8"], ["mk8"])
                TS("dve", den[:, 1:2], top8[:, 0:1], -1.0, ALU.mult, ["top8"], ["den"])
                ACT(ex8, lg, AF.Exp, ["lg", "den"], ["ex8"], bias=den[:, 1:2])
                TT("dve", ex8, ex8, mk8, ALU.mult, ["ex8", "mk8"], ["ex8"])
                P.add("dve", lambda h: h.tensor_reduce(out=den[:, 0:1], in_=ex8, axis=AX.X, op=ALU.add),
                      reads=["ex8"], writes=["den"])
                RECIP(den[:, 0:1], den[:, 0:1], ["den"], ["den"])
                TS("dve", cmb[:, jj, :], ex8, den[:, 0:1], ALU.mult, ["ex8", "den"], [("cmb", jj)])
        epoch()
        cbs = [alloc([128, OWN], F32) for _ in range(2)]
        dg = [alloc([128, 128], F32) for _ in range(2)]
        sg = [alloc([128, 256], F32) for _ in range(2)]
        tg = [alloc([128, 256], F32) for _ in range(2)]
        actE = [alloc([128, 4, 256], BF16) for _ in range(2)]

        def emit_cb(e):
            cb = cbs[e % 2]
            for jj in range(16):
                d_ = dg[jj % 2]
                TS("dve", d_, ident32, cmb[:, jj, e:e + 1], ALU.mult, ["ident32", ("cmb", jj)], [("dg", jj % 2)])
                MM(bank(7)[:, (jj % 4) * 128:(jj % 4 + 1) * 128], ones32, d_, True, True, ["ones32", ("dg", jj % 2)], BK(7))
                if jj % 4 == 3:
                    CP("act", cb[:, (jj - 3) * 128:(jj + 1) * 128], bank(7), BK(7), [("cb", e % 2, jj // 4)])

        def emit_gu(e, sa, ka, sb_, kb, ti):
            o0 = ti * 256
            ae = actE[ti % 2]
            cb = cbs[e % 2]
            for fb in range(4):
                bk = (ti * 4 + fb) % 4
                pg = bank(bk)[:, 0:256]
                pu = bank(bk)[:, 256:512]
                for k in range(8):
                    MM(pg, sa[:, k, fb * 128:(fb + 1) * 128], hn[:, k, o0:o0 + 256], k == 0, k == 7,
                       [ka, ("hn", k, o0 // 512)], BK(bk))
                for k in range(8):
                    MM(pu, sb_[:, k, fb * 128:(fb + 1) * 128], hn[:, k, o0:o0 + 256], k == 0, k == 7,
                       [kb, ("hn", k, o0 // 512)], BK(bk))
                s_ = sg[fb % 2]
                t_ = tg[fb % 2]
                ACT(s_, pg, AF.Silu, BK(bk), [("sg", fb % 2)])
                TT("dve", t_, s_, pu, ALU.mult, [("sg", fb % 2)] + BK(bk), [("tg", fb % 2)])
                TT("pool", ae[:, fb, :], t_, cb[:, o0:o0 + 256], ALU.mult, [("tg", fb % 2), ("cb", e % 2, o0 // 512)],
                   [("ae", ti % 2, fb)])

        def emit_down(sc, kc, ti):
            o0 = ti * 256
            ae = actE[ti % 2]
            for n in range(8):
                yb = 4 + n // 2
                yv = bank(yb)[:, (n % 2) * 256:(n % 2 + 1) * 256]
                for fb in range(4):
                    MM(yv, sc[:, fb, n * 128:(n + 1) * 128], ae[:, fb, :], fb == 0, fb == 3,
                       [kc, ("ae", ti % 2, fb)], BK(yb))
            for n2 in range(4):
                yv = bank(4 + n2).rearrange("p (a b) -> p a b", a=2)
                xv = xT[:, 2 * n2:2 * n2 + 2, 128 + o0:128 + o0 + 256]
                TT("dve", xv, yv, xv, ALU.add, BK(4 + n2) + xk([2 * n2, 2 * n2 + 1], 128 + o0, 256),
                   xk([2 * n2, 2 * n2 + 1], 128 + o0, 256))

        emit_cb(0)
        for e in range(NE):
            for fg in range(7):
                sa, ka = stA.next()
                sb_, kb = stB.next()
                sc, kc = stC.next()
                for ti in range(8):
                    emit_gu(e, sa, ka, sb_, kb, ti)
                    if ti > 0:
                        emit_down(sc, kc, ti - 1)
                    if fg == 3 and ti == 3 and e + 1 < NE:
                        emit_cb(e + 1)
                emit_down(sc, kc, 7)
        state["mark"] = mark_save5
        epoch()

    if stop_after >= 6:
        ple_stage(1, p1_in, tiles_own, 128, V["pleg1"])

    gb = alloc([128, D], F32)
    DMA("sp", gb, W["final_norm_g"].partition_broadcast(128), "dc", [], ["gb"])
    yo = [alloc([128, D], F32) for _ in range(2)]
    junk = alloc([128, D], F32)
    ss = alloc([128, 4], F32)
    outs = []
    for b in range(OWN // 128):
        t0 = 128 + b * 128
        pb = 4 * (b % 2)
        for half in range(2):
            for c in range(4):
                cc = half * 4 + c
                TR(PS[:, (pb + 2 * half) * 512 + c * 128:(pb + 2 * half) * 512 + (c + 1) * 128], xT[:, cc, t0:t0 + 128], ident32,
                   xk([cc], t0, 128) + ["ident32"], BK(pb + 2 * half))
        src = [PS[:, (pb + 2 * half) * 512:(pb + 2 * half) * 512 + 512] for half in range(2)]
        y_ = yo[b % 2]
        if final_norm:
            MS("dve", ss, 0.0, ["ss"])
            for half in range(2):
                ACT(junk[:, half * 512:(half + 1) * 512], src[half], AF.Square, BK(pb + 2 * half) + ["ss"], ["junk", "ss"],
                    accum=ss[:, half:half + 1])
            TT("dve", ss[:, 2:3], ss[:, 0:1], ss[:, 1:2], ALU.add, ["ss"], ["ss"])
            ACT(ss[:, 2:3], ss[:, 2:3], AF.Sqrt, ["ss", "epst"], ["ss"], bias=epst, scale=1.0 / D)
            RECIP(ss[:, 3:4], ss[:, 2:3], ["ss"], ["ss"])
            for half in range(2):
                STT("dve", y_[:, half * 512:(half + 1) * 512], src[half], ss[:, 3:4], gb[:, half * 512:(half + 1) * 512],
                    ALU.mult, ALU.mult, BK(pb + 2 * half) + ["ss", "gb"], [("yo", b % 2)])
        else:
            for half in range(2):
                CP("dve", y_[:, half * 512:(half + 1) * 512], src[half], BK(pb + 2 * half), [("yo", b % 2)])
        outs.append(DMA("sp", y_out[b * 128:(b + 1) * 128, :], y_, "dout" if b % 2 == 0 else "dout2", [("yo", b % 2)], []))

    P.emit_all(nc, sems, final_waits=outs[-2:])
    return nc, es


_CACHE = {}


def _core_inputs(x, p, c):
    b = c // 2
    half = c % 2
    s0 = half * OWN
    xc = np.zeros((NF, D), np.float32)
    p0c = np.zeros((NF, PLE), np.float32)
    if half == 1:
        xc[:] = x[b, s0 - HALO:s0 + OWN]
        p0c[:] = p[0, b, s0 - HALO:s0 + OWN]
    else:
        xc[HALO:] = x[b, 0:OWN]
        p0c[HALO:] = p[0, b, 0:OWN]
    p1c = np.ascontiguousarray(p[1, b, s0:s0 + OWN])
    pos = np.maximum(np.arange(s0 - HALO, s0 + OWN), 0).astype(np.float32)
    hm = np.full((128, 128), 0.0 if half == 1 else NEG, np.float32)
    return {"xc": xc, "p0c": p0c, "p1c": p1c, "pos": pos, "hm": hm}


def kernel(**inputs):
    x = np.asarray(inputs["x"], np.float32)
    p = np.asarray(inputs["p"], np.float32)
    if "nc" not in _CACHE:
        _CACHE["nc"] = build()
    nc, _es = _CACHE["nc"]
    wmap = {name: np.ascontiguousarray(np.asarray(inputs[name], np.float32)) for name, _ in WEIGHT_SPECS}
    in_maps = []
    for c in range(NCORES):
        m = dict(wmap)
        m.update(_core_inputs(x, p, c))
        in_maps.append(m)
    res = run_bass_kernel_spmd(nc, in_maps, core_ids=list(range(NCORES)))
    out = np.zeros((4, SEQ, D), np.float32)
    for c in range(NCORES):
        b, half = c // 2, c % 2
        out[b, half * OWN:(half + 1) * OWN] = res.results[c]["y"]
    return out
```
